# Optimizing a Trainium2 kernel written in Bass

```python
import math
import jax, jax.numpy as jnp
from jax import lax
import numpy as np

D_MODEL = 2048
BATCH = 2
SEQ = 8192
DEPTH = 1

HEAD_DIM = 128
MIX_WIDTH = D_MODEL
FOX_HEADS = MIX_WIDTH // 2 // HEAD_DIM
DIFF_HEADS = MIX_WIDTH // 2 // (2 * HEAD_DIM)
FOX_WIDTH = FOX_HEADS * HEAD_DIM
DIFF_WIDTH = DIFF_HEADS * 2 * HEAD_DIM
IN_SPLIT_SIZES = (FOX_WIDTH, FOX_WIDTH, FOX_WIDTH, FOX_HEADS, DIFF_WIDTH, DIFF_WIDTH, DIFF_WIDTH)
IN_COLS = sum(IN_SPLIT_SIZES)
Q_BLOCK = 128
N_GROUPS = 8
EXPERTS_PER_GROUP = 8
N_EXPERTS = N_GROUPS * EXPERTS_PER_GROUP
TOP_K_IN_GROUP = 2
D_FF = D_MODEL // 2
MOE_BLOCK = 128
NORM_EPS = 1e-6
SUBLN_EPS = 1e-5
FORGET_BIAS_CENTER = 3.0

kernel_name = "hybrid_fox_diffattn_hiermoe"


def rms_norm(x, g, eps=NORM_EPS):
    xf = x.astype(jnp.float32)
    y = xf * lax.rsqrt(jnp.mean(xf * xf, axis=-1, keepdims=True) + eps)
    return (y * g.astype(jnp.float32)).astype(x.dtype)


def alibi_slopes(n_heads):
    return jnp.asarray(2.0 ** (-8.0 * np.arange(1, n_heads + 1) / n_heads), dtype=jnp.float32)


def forgetting_attention(q, k, v, log_f):
    S = q.shape[2]
    scale = HEAD_DIM ** -0.5
    c = jnp.cumsum(log_f, axis=-1)
    outs = []
    for blk in range(S // Q_BLOCK):
        q0, q1 = blk * Q_BLOCK, (blk + 1) * Q_BLOCK
        logits = jnp.einsum('bhqd,bhkd->bhqk', q[:, :, q0:q1], k[:, :, :q1],
                            preferred_element_type=jnp.float32) * scale
        logits = logits + c[:, :, q0:q1, None] - c[:, :, None, :q1]
        mask = (q0 + jnp.arange(Q_BLOCK))[:, None] >= jnp.arange(q1)[None, :]
        p = jax.nn.softmax(jnp.where(mask, logits, -jnp.inf), axis=-1)
        outs.append(jnp.einsum('bhqk,bhkd->bhqd', p.astype(v.dtype), v[:, :, :q1]))
    return jnp.concatenate(outs, axis=2)


def differential_attention(q, k, v, lam, slopes):
    S = q.shape[3]
    scale = HEAD_DIM ** -0.5
    outs = []
    for blk in range(S // Q_BLOCK):
        q0, q1 = blk * Q_BLOCK, (blk + 1) * Q_BLOCK
        logits = jnp.einsum('bhiqd,bhikd->bhiqk', q[:, :, :, q0:q1], k[:, :, :, :q1],
                            preferred_element_type=jnp.float32) * scale
        dist = ((q0 + jnp.arange(Q_BLOCK))[:, None] - jnp.arange(q1)[None, :]).astype(jnp.float32)
        logits = logits - slopes[None, :, None, None, None] * dist
        p = jax.nn.softmax(jnp.where(dist >= 0, logits, -jnp.inf), axis=-1)
        w = p[:, :, 0] - lam * p[:, :, 1]
        outs.append(jnp.einsum('bhqk,bhkd->bhqd', w.astype(v.dtype), v[:, :, :q1]))
    return jnp.concatenate(outs, axis=2)


def hierarchical_moe(x, wg, bg, we, be, w_gate, w_up, w_down):
    B, S, D = x.shape
    N = B * S
    xf = x.reshape(N, D)
    group_probs = jax.nn.softmax((xf @ wg + bg).astype(jnp.float32), axis=-1)
    g_prob, g_idx = lax.top_k(group_probs, 1)
    exp_logits = (xf @ we + be).astype(jnp.float32).reshape(N, N_GROUPS, EXPERTS_PER_GROUP)
    idx = jnp.broadcast_to(g_idx[:, :, None], (N, 1, EXPERTS_PER_GROUP))
    in_group = jnp.take_along_axis(exp_logits, idx, axis=1)[:, 0]
    e_prob, e_local = lax.top_k(jax.nn.softmax(in_group, axis=-1), TOP_K_IN_GROUP)
    gates = g_prob * e_prob / jnp.sum(e_prob, axis=-1, keepdims=True)
    expert_id = g_idx * EXPERTS_PER_GROUP + e_local

    A = N * TOP_K_IN_GROUP
    e_flat = expert_id.reshape(A)
    tok_flat = jnp.repeat(jnp.arange(N, dtype=jnp.int32), TOP_K_IN_GROUP)
    gate_flat = gates.reshape(A)
    order = jnp.argsort(e_flat)
    e_sorted = e_flat[order]
    counts = jnp.bincount(e_flat, length=N_EXPERTS)
    starts = jnp.cumsum(counts) - counts
    padded = ((counts + MOE_BLOCK - 1) // MOE_BLOCK) * MOE_BLOCK
    pends = jnp.cumsum(padded)
    pstarts = pends - padded
    dest = pstarts[e_sorted] + (jnp.arange(A) - starts[e_sorted])
    n_rows = ((A + N_EXPERTS * (MOE_BLOCK - 1) + MOE_BLOCK - 1) // MOE_BLOCK) * MOE_BLOCK
    n_blocks = n_rows // MOE_BLOCK
    row_tok = jnp.full((n_rows,), N, jnp.int32).at[dest].set(tok_flat[order])
    row_gate = jnp.zeros((n_rows,), jnp.float32).at[dest].set(gate_flat[order])
    block_expert = jnp.minimum(
        jnp.searchsorted(pends, jnp.arange(n_blocks) * MOE_BLOCK, side='right'), N_EXPERTS - 1)
    x_pad = jnp.concatenate([xf, jnp.zeros((1, D), xf.dtype)], axis=0)
    x_rows = x_pad[row_tok].reshape(n_blocks, MOE_BLOCK, D)

    def expert_block(args):
        xb, e = args
        hdn = jax.nn.silu(xb @ w_gate[e]) * (xb @ w_up[e])
        return hdn @ w_down[e]

    y_rows = lax.map(expert_block, (x_rows, block_expert)).reshape(n_rows, D)
    y = jnp.zeros((N + 1, D), jnp.float32).at[row_tok].add(
        y_rows.astype(jnp.float32) * row_gate[:, None])
    return y[:N].astype(x.dtype).reshape(B, S, D)


def setup_inputs(seed: int = 0) -> dict:
    key = jax.random.key(seed)
    ks = jax.random.split(key, 20)
    L, D = DEPTH, D_MODEL
    nrm = lambda k, shape, s: jax.random.normal(k, shape, jnp.float32) * s
    return {
        "x": nrm(ks[0], (BATCH, SEQ, D), 1.0),
        "attn_norm_g": 1.0 + nrm(ks[1], (L, D), 0.02),
        "w_in": nrm(ks[2], (L, D, IN_COLS), D ** -0.5),
        "forget_bias": FORGET_BIAS_CENTER + nrm(ks[3], (L, FOX_HEADS), 0.5),
        "lambda_q1": nrm(ks[4], (L, HEAD_DIM), 0.1),
        "lambda_k1": nrm(ks[5], (L, HEAD_DIM), 0.1),
        "lambda_q2": nrm(ks[6], (L, HEAD_DIM), 0.1),
        "lambda_k2": nrm(ks[7], (L, HEAD_DIM), 0.1),
        "diff_subln_g": 1.0 + nrm(ks[8], (L, 2 * HEAD_DIM), 0.02),
        "w_out": nrm(ks[9], (L, MIX_WIDTH, D), MIX_WIDTH ** -0.5),
        "ffn_norm_g": 1.0 + nrm(ks[10], (L, D), 0.02),
        "router_group_w": nrm(ks[11], (L, D, N_GROUPS), D ** -0.5),
        "router_group_b": nrm(ks[12], (L, N_GROUPS), 0.01),
        "router_expert_w": nrm(ks[13], (L, D, N_EXPERTS), D ** -0.5),
        "router_expert_b": nrm(ks[14], (L, N_EXPERTS), 0.01),
        "w_gate": nrm(ks[15], (L, N_EXPERTS, D, D_FF), D ** -0.5),
        "w_up": nrm(ks[16], (L, N_EXPERTS, D, D_FF), D ** -0.5),
        "w_down": nrm(ks[17], (L, N_EXPERTS, D_FF, D), D_FF ** -0.5),
        "final_norm_g": 1.0 + nrm(ks[18], (D,), 0.02),
    }


def reference(x, attn_norm_g, w_in, forget_bias, lambda_q1, lambda_k1, lambda_q2, lambda_k2,
              diff_subln_g, w_out, ffn_norm_g, router_group_w, router_group_b,
              router_expert_w, router_expert_b, w_gate, w_up, w_down, final_norm_g):
    B, S, _ = x.shape
    slopes = alibi_slopes(DIFF_HEADS)
    split_points = [int(v) for v in np.cumsum(IN_SPLIT_SIZES)[:-1]]
    h = x
    for l in range(DEPTH):
        a = rms_norm(h, attn_norm_g[l])
        proj = a @ w_in[l]
        fq, fk, fv, ff, dq, dk, dv = jnp.split(proj, split_points, axis=-1)
        to_heads = lambda t: t.reshape(B, S, FOX_HEADS, HEAD_DIM).transpose(0, 2, 1, 3)
        log_f = jax.nn.log_sigmoid((ff + forget_bias[l]).astype(jnp.float32)).transpose(0, 2, 1)
        fox = forgetting_attention(to_heads(fq), to_heads(fk), to_heads(fv), log_f)
        fox = fox.transpose(0, 2, 1, 3).reshape(B, S, FOX_WIDTH)

        to_pair = lambda t: t.reshape(B, S, DIFF_HEADS, 2, HEAD_DIM).transpose(0, 2, 3, 1, 4)
        dvh = dv.reshape(B, S, DIFF_HEADS, 2 * HEAD_DIM).transpose(0, 2, 1, 3)
        lam_init = 0.8 - 0.6 * math.exp(-0.3 * l)
        f32 = lambda t: t.astype(jnp.float32)
        lam = (jnp.exp(jnp.sum(f32(lambda_q1[l]) * f32(lambda_k1[l])))
               - jnp.exp(jnp.sum(f32(lambda_q2[l]) * f32(lambda_k2[l]))) + lam_init)
        diff = differential_attention(to_pair(dq), to_pair(dk), dvh, lam, slopes)
        diff = rms_norm(diff, diff_subln_g[l], SUBLN_EPS) * (1.0 - lam_init)
        diff = diff.transpose(0, 2, 1, 3).reshape(B, S, DIFF_WIDTH)

        h = h + jnp.concatenate([fox, diff], axis=-1) @ w_out[l]
        h = h + hierarchical_moe(rms_norm(h, ffn_norm_g[l]), router_group_w[l], router_group_b[l],
                                 router_expert_w[l], router_expert_b[l],
                                 w_gate[l], w_up[l], w_down[l])
    return rms_norm(h, final_norm_g)
```

```python
from contextlib import ExitStack
import numpy as np
import ml_dtypes
import concourse.bass as bass
import concourse.mybir as mybir
from concourse.bass_utils import run_bass_kernel_spmd

F32 = mybir.dt.float32
BF16 = mybir.dt.bfloat16
I32 = mybir.dt.int32
AF = mybir.ActivationFunctionType
ALU = mybir.AluOpType
AX = mybir.AxisListType
NPBF = ml_dtypes.bfloat16

D = 2048
S = 8192
NB = 2
NCORES = 8
HD = 128
NWC = 1538
SCALE = HD ** -0.5
LAM_INIT = 0.2
ENGS = ("pe", "act", "dve", "pool", "sp")
NDMASEM = 8


class Prog:
    def __init__(self, nc):
        self.nc = nc
        self.es = ExitStack()
        self.ops = {e: [] for e in ENGS}
        self.cnt = {e: 0 for e in ENGS}
        self.sem = {e: self.es.enter_context(nc.semaphore("s_" + e)) for e in ENGS}
        self.dsem = {q: [self.es.enter_context(nc.semaphore(f"d_{q}{i}")) for i in range(NDMASEM)]
                     for q in ("sp", "pool")}
        self.dcnt = {"sp": 0, "pool": 0}
        self.known = {e: {} for e in ENGS}
        self.lastw = {}
        self.readers = {}
        self.final_waits = []
        self.phase_es = None

    def begin_phase(self):
        self.phase_es = ExitStack()

    def sb(self, name, shape, dt):
        return self.phase_es.enter_context(self.nc.sbuf_tensor("sb_" + name, list(shape), dt))

    def ps(self, name, shape, dt):
        return self.phase_es.enter_context(self.nc.psum_tensor("ps_" + name, list(shape), dt))

    def _deps(self, eng, reads, writes):
        deps = set()
        for r in reads:
            w = self.lastw.get(r)
            if w is not None:
                deps.add(w)
        for w_ in writes:
            w = self.lastw.get(w_)
            if w is not None:
                deps.add(w)
            for rd in self.readers.get(w_, ()):
                deps.add(rd)
        best = {}
        for d in deps:
            if d[0] == "pe" and eng == "pe":
                continue
            if self.known[eng].get(d[0], 0) >= d[1]:
                continue
            if best.get(d[0], 0) < d[1]:
                best[d[0]] = d[1]
        for s, v in best.items():
            self.known[eng][s] = v
        return list(best.items())

    def _mark(self, token, reads, writes):
        for r in reads:
            self.readers.setdefault(r, []).append(token)
        for w in writes:
            self.lastw[w] = token
            self.readers[w] = []

    def op(self, eng, fn, reads=(), writes=(), after=()):
        waits = self._deps(eng, reads, writes)
        for (src, v) in after:
            if self.known[eng].get(src, 0) < v:
                waits.append((src, v))
                self.known[eng][src] = v
        self.cnt[eng] += 1
        token = (eng, self.cnt[eng])
        self.ops[eng].append((waits, fn, ("eng", eng)))
        self._mark(token, reads, writes)
        return token

    def dma(self, q, fn, reads=(), writes=(), is_output=False, inc=16):
        waits = self._deps(q, reads, writes)
        i = self.dcnt[q]
        self.dcnt[q] += 1
        slot = i % NDMASEM
        val = 16 * (i // NDMASEM + 1)
        src = ("d", q, slot)
        if i >= NDMASEM and self.known[q].get(src, 0) < val - 16:
            waits.append((src, val - 16))
            self.known[q][src] = val - 16
        token = (src, val)
        self.ops[q].append((waits, fn, ("dma", q, slot, inc)))
        self._mark(token, reads, writes)
        if is_output:
            self.final_waits.append(token)
        return token

    def _semof(self, src):
        if isinstance(src, tuple):
            return self.csem[src[1]] if src[0] == "c" else self.dsem[src[1]][src[2]]
        return self.sem[src]

    def collective(self, fn, writes=(), inc=16):
        if not hasattr(self, "csem"):
            self.csem = []
        sem = self.es.enter_context(self.nc.semaphore(f"cc{len(self.csem)}"))
        self.csem.append(sem)
        k = len(self.csem) - 1
        fn(self.nc.gpsimd).then_inc(sem, inc)
        self.nc.gpsimd.wait_ge(sem, inc)
        token = (("c", k), inc)
        self._mark(token, (), writes)
        return token

    def end_phase(self, final=False):
        allw = [(e, self.cnt[e]) for e in ENGS if self.cnt[e] > 0]
        for q in ("sp", "pool"):
            n = self.dcnt[q]
            for slot in range(min(n, NDMASEM)):
                last_i = ((n - 1 - slot) // NDMASEM) * NDMASEM + slot
                allw.append((("d", q, slot), 16 * (last_i // NDMASEM + 1)))
        for e in ENGS:
            w = [(s, v) for (s, v) in allw if self.known[e].get(s, 0) < v]
            for s, v in w:
                self.known[e][s] = v
            self.ops[e].append((w, None, None))
        nc = self.nc
        with nc.Block() as block:
            def run(eng_name):
                def body(e):
                    for waits, fn, inc in self.ops[eng_name]:
                        for (src, val) in waits:
                            e.wait_ge(self._semof(src), val)
                        if fn is None:
                            continue
                        ins = fn(e)
                        if inc[0] == "eng":
                            ins.then_inc(self.sem[inc[1]], 1)
                        else:
                            ins.then_inc(self.dsem[inc[1]][inc[2]], inc[3])
                return body
            block.tensor(run("pe"))
            block.scalar(run("act"))
            block.vector(run("dve"))
            block.gpsimd(run("pool"))
            block.sync(run("sp"))
        self.ops = {e: [] for e in ENGS}
        self.phase_es.close()
        self.phase_es = None
        if final:
            self.es.close()


def phase_proj(P, nc, x, wc, gcol, ident_d, qkT, vdr, ffd):
    P.begin_phase()
    wb = P.sb("wb", [128, 16, NWC], BF16)
    wst = [P.sb(f"wst{i}", [128, NWC], F32) for i in range(2)]
    gc = P.sb("gc", [128, 16], F32)
    ident = P.sb("ident", [128, 128], BF16)
    xt = [P.sb(f"xt{i}", [128, D], F32) for i in range(3)]
    junk = P.sb("junk", [128, D], BF16)
    ab = [P.sb(f"ab{i}", [128, D], BF16) for i in range(2)]
    ss = [P.sb(f"ss{i}", [128, 1], F32) for i in range(2)]
    rstd = [P.sb(f"rstd{i}", [128, 1], F32) for i in range(2)]
    aT = [P.sb(f"aT{i}", [128, 16, 512], BF16) for i in range(2)]
    qkst = [P.sb(f"qkst{i}", [128, 8, 512], BF16) for i in range(2)]
    vst = [P.sb(f"vst{i}", [128, 512], BF16) for i in range(2)]
    ffs = P.sb("ffs", [128, 128], F32)
    ptr = P.ps("ptr", [128, D], BF16)
    pqk = P.ps("pqk", [128, 2, 512], F32)
    pv = P.ps("pv", [128, 2, 512], F32)
    pf = P.ps("pf", [128, 2, 512], F32)

    P.dma("sp", lambda e: e.dma_start(out=gc[:], in_=gcol[:, :]), writes=["gc"])
    P.dma("sp", lambda e: e.dma_start(out=ident[:], in_=ident_d[:, :]), writes=["ident"])
    for c in range(16):
        st = wst[c % 2]
        P.dma("sp", lambda e, st=st, c=c: e.dma_start(out=st[:], in_=wc[c * 128:(c + 1) * 128, :]),
              writes=[f"wst{c % 2}"])
        P.op("pool", lambda e, st=st, c=c: e.tensor_scalar(out=wb[:, c, :], in0=st[:], scalar1=gc[:, c:c + 1],
                                                          scalar2=None, op0=ALU.mult),
             reads=[f"wst{c % 2}", "gc"], writes=[("wb", c)])
    wb_all = [("wb", c) for c in range(16)]

    tile_i = 0
    for gi in range(16):
        aTg = aT[gi % 2]
        aTn = f"aT{gi % 2}"
        for tt in range(4):
            ti = gi * 4 + tt
            xs = xt[ti % 3]; xn = f"xt{ti % 3}"
            abt = ab[ti % 2]; abn = f"ab{ti % 2}"
            sst = ss[ti % 2]; ssn = f"ss{ti % 2}"
            rs = rstd[ti % 2]; rsn = f"rstd{ti % 2}"
            P.dma("sp", lambda e, xs=xs, ti=ti: e.dma_start(out=xs[:], in_=x[ti * 128:(ti + 1) * 128, :]), writes=[xn])
            P.op("act", lambda e, xs=xs, sst=sst: e.activation(out=junk[:], in_=xs[:], func=AF.Square, accum_out=sst[:]),
                 reads=[xn], writes=["junk", ssn])
            P.op("act", lambda e, sst=sst: e.activation(out=sst[:], in_=sst[:], func=AF.Sqrt, bias=1e-6, scale=1.0 / D),
                 reads=[ssn], writes=[ssn])
            P.op("dve", lambda e, sst=sst, rs=rs: e.reciprocal(out=rs[:], in_=sst[:]), reads=[ssn], writes=[rsn])
            P.op("dve", lambda e, xs=xs, rs=rs, abt=abt: e.tensor_scalar(out=abt[:], in0=xs[:], scalar1=rs[:], scalar2=None, op0=ALU.mult),
                 reads=[xn, rsn], writes=[abn])
            for c in range(16):
                P.op("pe", lambda e, c=c, abt=abt: e.transpose(ptr[:, c * 128:(c + 1) * 128], abt[:, c * 128:(c + 1) * 128], ident[:]),
                     reads=[abn, "ident"], writes=[("ptr", c // 8)])
            for hh in range(2):
                eng = "act" if hh == 0 else "dve"
                def cp(e, hh=hh, aTg=aTg, tt=tt, eng=eng):
                    o = aTg[:, hh * 8:(hh + 1) * 8, tt * 128:(tt + 1) * 128]
                    i = ptr[:, hh * 1024:(hh + 1) * 1024].rearrange("p (c t) -> p c t", c=8)
                    if eng == "act":
                        return e.copy(out=o, in_=i)
                    return e.tensor_copy(out=o, in_=i)
                P.op(eng, cp, reads=[("ptr", hh)], writes=[(aTn, tt)])
        aT_all = [(aTn, tt) for tt in range(4)]
        qs = qkst[gi % 2]; qsn = f"qkst{gi % 2}"
        for cb in range(8):
            bank = cb % 2
            for c in range(16):
                P.op("pe", lambda e, cb=cb, c=c, bank=bank, aTg=aTg: e.matmul(pqk[:, bank, :], lhsT=wb[:, c, cb * 128:(cb + 1) * 128],
                                                                              rhs=aTg[:, c, :], start=(c == 0), stop=(c == 15)),
                     reads=wb_all + aT_all, writes=[("pqk", bank)])
            eng = "act" if cb % 2 == 0 else "dve"
            def ev(e, cb=cb, bank=bank, qs=qs, eng=eng):
                if eng == "act":
                    return e.copy(out=qs[:, cb, :], in_=pqk[:, bank, :])
                return e.tensor_copy(out=qs[:, cb, :], in_=pqk[:, bank, :])
            P.op(eng, ev, reads=[("pqk", bank)], writes=[(qsn, cb)])
        P.dma("pool", lambda e, qs=qs, gi=gi: e.dma_start(out=qkT[:, :, gi * 512:(gi + 1) * 512].rearrange("c p t -> p c t"), in_=qs[:]),
              reads=[(qsn, cb) for cb in range(8)], writes=[("qkT", gi)])
        for tt in range(4):
            ti = gi * 4 + tt
            bank = ti % 2
            vs = vst[ti % 2]; vsn = f"vst{ti % 2}"
            for c in range(16):
                P.op("pe", lambda e, c=c, bank=bank, tt=tt, aTg=aTg: e.matmul(pv[:, bank, :], lhsT=aTg[:, c, tt * 128:(tt + 1) * 128],
                                                                              rhs=wb[:, c, 1024:1536], start=(c == 0), stop=(c == 15)),
                     reads=wb_all + aT_all, writes=[("pv", bank)])
            for c in range(16):
                P.op("pe", lambda e, c=c, bank=bank, tt=tt, aTg=aTg: e.matmul(pf[:, bank, 0:2], lhsT=aTg[:, c, tt * 128:(tt + 1) * 128],
                                                                              rhs=wb[:, c, 1536:1538], start=(c == 0), stop=(c == 15)),
                     reads=wb_all + aT_all, writes=[("pf", bank)])
            eng = "act" if tt % 2 == 0 else "dve"
            def evv(e, bank=bank, vs=vs, eng=eng):
                if eng == "act":
                    return e.copy(out=vs[:], in_=pv[:, bank, :])
                return e.tensor_copy(out=vs[:], in_=pv[:, bank, :])
            P.op(eng, evv, reads=[("pv", bank)], writes=[vsn])
            P.op("dve", lambda e, bank=bank, ti=ti: e.tensor_copy(out=ffs[:, ti * 2:ti * 2 + 2], in_=pf[:, bank, 0:2]),
                 reads=[("pf", bank)], writes=[("ffs", ti)])
            P.dma("pool", lambda e, vs=vs, ti=ti: e.dma_start(out=vdr[ti * 128:(ti + 1) * 128, :], in_=vs[:]),
                  reads=[vsn], writes=[("vdr", ti)])
    P.dma("pool", lambda e: e.dma_start(out=ffd[:, :], in_=ffs[:]), reads=[("ffs", ti) for ti in range(64)], writes=["ffd"])
    P.end_phase()


def phase_attn(P, nc, qkT, vdr, ffd, fbias, alibi_d, tri_d, ones_d, lamv, sublng, o_out, out_is_final, nq=32, do_fox=(0, 1), do_diff=True, dbg=None):
    P.begin_phase()
    KT = P.sb("KT", [128, 4, S], BF16)
    Vf = [P.sb(f"Vf{h}", [128, 64, 129], BF16) for h in range(2)]
    Vd = P.sb("Vd", [128, 64, 257], BF16)
    G = P.sb("G", [128, 32, 64], F32)
    tri = P.sb("tri", [128, 128], F32)
    trib = P.sb("trib", [128, 128], BF16)
    onesf = P.sb("onesf", [128, 128], F32)
    ff = P.sb("ff", [128, 128], F32)
    fb = P.sb("fb", [128, 2], F32)
    lf = P.sb("lf", [128, 64], F32)
    ccol = P.sb("ccol", [128, 64], F32)
    sc = [P.sb(f"sc{i}", [128, 64], F32) for i in range(2)]
    ex = P.sb("ex", [128, 64], F32)
    lv = P.sb("lv", [128, 4, 128], F32)
    lsc = P.sb("lsc", [128, 128], F32)
    l1 = P.sb("l1", [128, 1], F32)
    l2 = P.sb("l2", [128, 1], F32)
    neglam = P.sb("neglam", [128, 1], F32)
    gs = P.sb("gs", [128, 256], F32)
    qt_ = [P.sb(f"qt{i}", [128, 256], BF16) for i in range(4)]
    pt = [P.sb(f"pt{i}", [128, 256], BF16) for i in range(4)]
    ost = [P.sb(f"ost{i}", [128, 2, 256], BF16) for i in range(2)]
    rec = [P.sb(f"rec{i}", [128, 1], F32) for i in range(4)]
    t0 = [P.sb(f"t0{i}", [128, 256], F32) for i in range(2)]
    t1 = [P.sb(f"t1{i}", [128, 256], F32) for i in range(2)]
    junk2 = P.sb("junk2", [128, 256], F32)
    ssd = [P.sb(f"ssd{i}", [128, 1], F32) for i in range(2)]
    ps_s = P.ps("ps_s", [128, 3, 512], F32)
    po = P.ps("po", [128, 4, 512], F32)
    pc = P.ps("pc", [128, 2, 64], F32)

    for i in range(4):
        P.dma("sp", lambda e, i=i: e.dma_start(out=KT[:, i, :], in_=qkT[4 + i, :, :]), writes=[("KT", i)])
    vview = vdr.rearrange("(k p) c -> p k c", p=128)
    for h in range(2):
        for kq in range(4):
            P.dma("sp", lambda e, h=h, kq=kq: e.dma_start(out=Vf[h][:, kq * 16:(kq + 1) * 16, 0:128],
                                                         in_=vview[:, kq * 16:(kq + 1) * 16, h * 128:(h + 1) * 128]), writes=[(f"Vf{h}", kq)])
        P.op("pool", lambda e, h=h: e.memset(Vf[h][:, :, 128:129], 1.0), reads=[(f"Vf{h}", kq) for kq in range(4)], writes=[f"Vf{h}o"])
    for kq in range(4):
        P.dma("sp", lambda e, kq=kq: e.dma_start(out=Vd[:, kq * 16:(kq + 1) * 16, 0:256], in_=vview[:, kq * 16:(kq + 1) * 16, 256:512]), writes=[("Vd", kq)])
    P.op("pool", lambda e: e.memset(Vd[:, :, 256:257], 1.0), reads=[("Vd", kq) for kq in range(4)], writes=["Vdo"])
    P.dma("sp", lambda e: e.dma_start(out=tri[:], in_=tri_d[:, :]), writes=["tri"])
    P.dma("sp", lambda e: e.dma_start(out=onesf[:], in_=ones_d[:, :]), writes=["onesf"])
    P.dma("sp", lambda e: e.dma_start(out=ff[:], in_=ffd[:, :]), writes=["ff"])
    P.dma("sp", lambda e: e.dma_start(out=fb[:], in_=fbias[0:1, :].broadcast_to([128, 2])), writes=["fb"])
    P.dma("sp", lambda e: e.dma_start(out=lv[:], in_=lamv[0:1, :, :].broadcast_to([128, 4, 128])), writes=["lv"])
    P.dma("sp", lambda e: e.dma_start(out=gs[:], in_=sublng[0:1, :].broadcast_to([128, 256])), writes=["gs"])
    P.op("dve", lambda e: e.tensor_copy(out=trib[:], in_=tri[:]), reads=["tri"], writes=["trib"])
    P.op("dve", lambda e: e.tensor_scalar(out=fb[:], in0=fb[:], scalar1=-1.0, scalar2=None, op0=ALU.mult), reads=["fb"], writes=["fb"])
    P.op("dve", lambda e: e.tensor_tensor(out=lsc[:], in0=lv[:, 0, :], in1=lv[:, 1, :], op=ALU.mult), reads=["lv"], writes=["lsc"])
    P.op("dve", lambda e: e.tensor_reduce(out=l1[:], in_=lsc[:], axis=AX.X, op=ALU.add), reads=["lsc"], writes=["l1"])
    P.op("dve", lambda e: e.tensor_tensor(out=lsc[:], in0=lv[:, 2, :], in1=lv[:, 3, :], op=ALU.mult), reads=["lv", "l1"], writes=["lsc"])
    P.op("dve", lambda e: e.tensor_reduce(out=l2[:], in_=lsc[:], axis=AX.X, op=ALU.add), reads=["lsc"], writes=["l2"])
    P.op("act", lambda e: e.activation(out=l1[:], in_=l1[:], func=AF.Exp), reads=["l1"], writes=["l1"])
    P.op("act", lambda e: e.activation(out=l2[:], in_=l2[:], func=AF.Exp), reads=["l2"], writes=["l2"])
    P.op("dve", lambda e: e.tensor_tensor(out=neglam[:], in0=l2[:], in1=l1[:], op=ALU.subtract), reads=["l1", "l2"], writes=["neglam"])
    P.op("dve", lambda e: e.tensor_scalar(out=neglam[:], in0=neglam[:], scalar1=-LAM_INIT, scalar2=None, op0=ALU.add), reads=["neglam"], writes=["neglam"])
    P.op("dve", lambda e: e.tensor_scalar(out=gs[:], in0=gs[:], scalar1=1.0 - LAM_INIT, scalar2=None, op0=ALU.mult), reads=["gs"], writes=["gs"])

    state = {"q": 0, "p": 0, "s": 0, "o": 0, "r": 0}

    def fox_table(h):
        P.op("act", lambda e: e.activation(out=lf[:], in_=ff[:].rearrange("p (k h) -> p k h", h=2)[:, :, h], func=AF.Exp,
                                           bias=fb[:, h:h + 1], scale=-1.0), reads=["ff", "fb"], writes=["lf"])
        P.op("act", lambda e: e.activation(out=lf[:], in_=lf[:], func=AF.Ln, bias=1.0, scale=1.0), reads=["lf"], writes=["lf"])
        P.op("dve", lambda e: e.tensor_scalar(out=lf[:], in0=lf[:], scalar1=-1.0, scalar2=None, op0=ALU.mult), reads=["lf"], writes=["lf"])
        P.op("pe", lambda e: e.matmul(pc[:, 0, :], lhsT=tri[:], rhs=lf[:], start=True, stop=True), reads=["tri", "lf"], writes=["pc0", "pcb"])
        state["f32mm"] = P.op("pe", lambda e: e.matmul(pc[:, 1, :], lhsT=onesf[:], rhs=lf[:], start=True, stop=True), reads=["onesf", "lf"], writes=["pc1", "pcb"])
        P.op("dve", lambda e: e.tensor_copy(out=sc[0][:], in_=pc[:, 1, :]), reads=["pc1", "pcb"], writes=["sc0"])
        cur = 0
        dd = 1
        while dd < 64:
            a, b = sc[cur], sc[1 - cur]
            an, bn = f"sc{cur}", f"sc{1 - cur}"
            P.op("dve", lambda e, a=a, b=b, dd=dd: e.tensor_copy(out=b[:, 0:dd], in_=a[:, 0:dd]), reads=[an], writes=[bn])
            P.op("dve", lambda e, a=a, b=b, dd=dd: e.tensor_tensor(out=b[:, dd:64], in0=a[:, dd:64], in1=a[:, 0:64 - dd], op=ALU.add),
                 reads=[an, bn], writes=[bn])
            cur = 1 - cur
            dd *= 2
        inc = sc[cur]; incn = f"sc{cur}"
        P.op("dve", lambda e, inc=inc: e.tensor_tensor(out=ex[:], in0=inc[:], in1=pc[:, 1, :], op=ALU.subtract), reads=[incn, "pc1", "pcb"], writes=["ex"])
        P.op("dve", lambda e: e.tensor_tensor(out=ccol[:], in0=pc[:, 0, :], in1=ex[:], op=ALU.add), reads=["pc0", "pcb", "ex"], writes=["ccol"])
        for qt in range(32):
            P.op("dve", lambda e, qt=qt: e.tensor_scalar(out=G[:, qt, :], in0=ccol[:], scalar1=-1.0, scalar2=ex[:, 2 * qt:2 * qt + 1],
                                                         op0=ALU.mult, op1=ALU.add), reads=["ccol", "ex"], writes=[("G", qt)])

    def run_maps(maps, V, vname, dv, epilogue, nq=32):
        nm = len(maps)
        for qt in range(nq):
            qtiles = []
            for (ki, qi) in maps:
                s = state["q"] % 4; state["q"] += 1
                P.dma("sp", lambda e, s=s, qi=qi, qt=qt: e.dma_start(out=qt_[s][:], in_=qkT[qi, :, qt * 256:(qt + 1) * 256]), writes=[f"qt{s}"])
                qtiles.append(s)
            nkb = 2 * qt + 2
            steps = [(kb, m) for kb in range(nkb) for m in range(nm)]
            sslot = {}
            def issue_S(idx):
                kb, m = steps[idx]
                s = state["s"] % 3; state["s"] += 1
                sslot[idx] = s
                ki = maps[m][0]
                lo = 128 if kb == nkb - 1 else 0
                qb = qtiles[m]
                P.op("pe", lambda e, s=s, ki=ki, kb=kb, qb=qb, lo=lo: e.matmul(ps_s[:, s, lo:256], lhsT=KT[:, ki, kb * 128:(kb + 1) * 128],
                                                                              rhs=qt_[qb][:, lo:256], start=True, stop=True),
                     reads=[("KT", ki), f"qt{qb}"], writes=[("ps_s", s)], after=[state["f32mm"]] if "f32mm" in state else [])
            LA = 2
            for idx in range(min(LA, len(steps))):
                issue_S(idx)
            for idx, (kb, m) in enumerate(steps):
                if idx + LA < len(steps):
                    issue_S(idx + LA)
                s = sslot[idx]
                p = state["p"] % 4; state["p"] += 1
                lo = 128 if kb == nkb - 1 else 0
                P.op("act", lambda e, s=s, p=p, kb=kb, qt=qt, lo=lo: e.activation(out=pt[p][:, lo:256], in_=ps_s[:, s, lo:256], func=AF.Exp,
                                                                                bias=G[:, qt, kb:kb + 1], scale=SCALE),
                     reads=[("ps_s", s), ("G", qt)], writes=[f"pt{p}"])
                if kb >= nkb - 2:
                    j = kb - (nkb - 2)
                    P.op("dve", lambda e, p=p, j=j: e.tensor_tensor(out=pt[p][:, j * 128:(j + 1) * 128], in0=pt[p][:, j * 128:(j + 1) * 128],
                                                                    in1=trib[:], op=ALU.mult), reads=[f"pt{p}", "trib"], writes=[f"pt{p}"])
                for j in range(2):
                    if j == 0 and kb == nkb - 1:
                        continue
                    last = (kb == nkb - 2) if j == 0 else (kb == nkb - 1)
                    r = m * 2 + j
                    P.op("pe", lambda e, p=p, j=j, kb=kb, r=r, last=last: e.matmul(po[:, r, 0:dv + 1], lhsT=pt[p][:, j * 128:(j + 1) * 128],
                                                                                   rhs=V[:, kb, :], start=(kb == 0), stop=last),
                         reads=[f"pt{p}", (vname, kb // 16), vname + "o"], writes=[("po", r)])
            epilogue(qt)

    def fox_epilogue(h):
        def ep(qt):
            o = state["o"] % 2; state["o"] += 1
            for j in range(2):
                r = state["r"] % 4; state["r"] += 1
                P.op("dve", lambda e, r=r, j=j: e.reciprocal(out=rec[r][:], in_=po[:, j, 128:129]), reads=[("po", j)], writes=[f"rec{r}"])
                P.op("dve", lambda e, r=r, j=j, o=o: e.tensor_scalar(out=ost[o][:, j, 0:128], in0=po[:, j, 0:128], scalar1=rec[r][:], scalar2=None,
                                                                     op0=ALU.mult), reads=[("po", j), f"rec{r}"], writes=[f"ost{o}"])
            P.dma("pool", lambda e, o=o, qt=qt: e.dma_start(
                out=o_out[qt * 256:(qt + 1) * 256, h * 128:(h + 1) * 128].rearrange("(j p) c -> p j c", p=128), in_=ost[o][:, :, 0:128]),
                reads=[f"ost{o}"], writes=[("o_out", h, qt)], is_output=out_is_final)
        return ep

    def diff_epilogue(qt):
        o = state["o"] % 2; state["o"] += 1
        for j in range(2):
            r0 = state["r"] % 4; state["r"] += 1
            r1 = state["r"] % 4; state["r"] += 1
            P.op("dve", lambda e, r0=r0, j=j: e.reciprocal(out=rec[r0][:], in_=po[:, j, 256:257]), reads=[("po", j)], writes=[f"rec{r0}"])
            P.op("dve", lambda e, r1=r1, j=j: e.reciprocal(out=rec[r1][:], in_=po[:, 2 + j, 256:257]), reads=[("po", 2 + j)], writes=[f"rec{r1}"])
            P.op("dve", lambda e, r1=r1: e.tensor_tensor(out=rec[r1][:], in0=rec[r1][:], in1=neglam[:], op=ALU.mult), reads=[f"rec{r1}", "neglam"], writes=[f"rec{r1}"])
            P.op("dve", lambda e, r0=r0, j=j: e.tensor_scalar(out=t0[j][:], in0=po[:, j, 0:256], scalar1=rec[r0][:], scalar2=None, op0=ALU.mult),
                 reads=[("po", j), f"rec{r0}"], writes=[f"t0{j}"])
            P.op("dve", lambda e, r1=r1, j=j: e.scalar_tensor_tensor(out=t1[j][:], in0=po[:, 2 + j, 0:256], scalar=rec[r1][:], in1=t0[j][:],
                                                                     op0=ALU.mult, op1=ALU.add), reads=[("po", 2 + j), f"rec{r1}", f"t0{j}"], writes=[f"t1{j}"])
            P.op("act", lambda e, j=j: e.activation(out=junk2[:], in_=t1[j][:], func=AF.Square, accum_out=ssd[j][:]), reads=[f"t1{j}"], writes=["junk2", f"ssd{j}"])
            P.op("act", lambda e, j=j: e.activation(out=ssd[j][:], in_=ssd[j][:], func=AF.Sqrt, bias=1e-5, scale=1.0 / 256), reads=[f"ssd{j}"], writes=[f"ssd{j}"])
            P.op("dve", lambda e, j=j: e.reciprocal(out=ssd[j][:], in_=ssd[j][:]), reads=[f"ssd{j}"], writes=[f"ssd{j}"])
            P.op("dve", lambda e, j=j, o=o: e.scalar_tensor_tensor(out=ost[o][:, j, :], in0=t1[j][:], scalar=ssd[j][:], in1=gs[:], op0=ALU.mult, op1=ALU.mult),
                 reads=[f"t1{j}", f"ssd{j}", "gs"], writes=[f"ost{o}"])
        P.dma("pool", lambda e, o=o, qt=qt: e.dma_start(
            out=o_out[qt * 256:(qt + 1) * 256, 256:512].rearrange("(j p) c -> p j c", p=128), in_=ost[o][:]),
            reads=[f"ost{o}"], writes=[("o_out", 2, qt)], is_output=out_is_final)

    Gall = [("G", qt) for qt in range(32)]
    for h in do_fox:
        fox_table(h)
        run_maps([(h, h)], Vf[h], f"Vf{h}", 128, fox_epilogue(h), nq=nq)
    if do_diff:
        P.dma("sp", lambda e: e.dma_start(out=G[:].rearrange("p a b -> p (a b)"), in_=alibi_d[:, :]), writes=Gall)
    if dbg is not None:
        P.dma("sp", lambda e: e.dma_start(out=dbg[:, 0:2048], in_=G[:].rearrange("p a b -> p (a b)")), reads=Gall, writes=["dbg0"], is_output=True)
        P.dma("sp", lambda e: e.dma_start(out=dbg[:, 2048:2112], in_=ccol[:]), reads=["ccol"], writes=["dbg1"], is_output=True)
        P.dma("sp", lambda e: e.dma_start(out=dbg[:, 2112:2176], in_=ex[:]), reads=["ex"], writes=["dbg2"], is_output=True)
        P.dma("sp", lambda e: e.dma_start(out=dbg[:, 2176:2240], in_=lf[:]), reads=["lf"], writes=["dbg3"], is_output=True)
    if do_diff:
        run_maps([(2, 2), (3, 3)], Vd, "Vd", 256, diff_epilogue, nq=nq)
    P.end_phase(final=out_is_final)


def build_l1(debug=False, only_proj=False):
    nc = bass.Bass("TRN2", target_bir_lowering=False)
    x = nc.dram_tensor("x", [S, D], F32, kind="ExternalInput").ap()
    wc = nc.dram_tensor("wc", [D, NWC], F32, kind="ExternalInput").ap()
    gcol = nc.dram_tensor("gcol", [128, 16], F32, kind="ExternalInput").ap()
    ident = nc.dram_tensor("ident", [128, 128], BF16, kind="ExternalInput").ap()
    fbias = nc.dram_tensor("fbias", [1, 2], F32, kind="ExternalInput").ap()
    alibi = nc.dram_tensor("alibi", [128, 2048], F32, kind="ExternalInput").ap()
    tri = nc.dram_tensor("tri", [128, 128], F32, kind="ExternalInput").ap()
    ones = nc.dram_tensor("ones", [128, 128], F32, kind="ExternalInput").ap()
    lamv = nc.dram_tensor("lamv", [1, 4, 128], F32, kind="ExternalInput").ap()
    sublng = nc.dram_tensor("sublng", [1, 256], F32, kind="ExternalInput").ap()
    kind = "ExternalOutput" if debug else "Internal"
    qkT = nc.dram_tensor("qkT", [8, 128, S], BF16, kind=kind).ap()
    vdr = nc.dram_tensor("vdr", [S, 512], BF16, kind=kind).ap()
    ffd = nc.dram_tensor("ffd", [128, 128], F32, kind=kind).ap()
    o = nc.dram_tensor("o", [S, 512], BF16, kind="ExternalOutput").ap()
    P = Prog(nc)
    phase_proj(P, nc, x, wc, gcol, ident, qkT, vdr, ffd)
    if only_proj:
        P.es.close()
        return nc
    phase_attn(P, nc, qkT, vdr, ffd, fbias, alibi, tri, ones, lamv, sublng, o, True)
    return nc


def l1_inputs(inp):
    x = inp["x"]
    w_in = inp["w_in"][0]
    offs = np.cumsum([0, 1024, 1024, 1024, 8, 1024, 1024, 1024])
    g = inp["attn_norm_g"][0]
    gcol = np.ascontiguousarray(g.reshape(16, 128).T)
    ident = np.eye(128, dtype=np.float32).astype(NPBF)
    tri = np.triu(np.ones((128, 128), np.float32))
    ones = np.ones((128, 128), np.float32)
    lamv = np.stack([inp["lambda_q1"][0], inp["lambda_k1"][0], inp["lambda_q2"][0], inp["lambda_k2"][0]])[None]
    slopes = 2.0 ** (-8.0 * np.arange(1, 5) / 4)
    p = np.arange(128, dtype=np.float32)[:, None, None]
    qt = np.arange(32, dtype=np.float32)[None, :, None]
    kb = np.arange(64, dtype=np.float32)[None, None, :]
    rel = kb * 128 + p - qt * 256
    maps = []
    for c in range(NCORES):
        b, j = c // 4, c % 4
        cols = np.concatenate([
            np.arange(offs[0] + 2 * j * 128, offs[0] + (2 * j + 2) * 128), np.arange(offs[4] + j * 256, offs[4] + (j + 1) * 256),
            np.arange(offs[1] + 2 * j * 128, offs[1] + (2 * j + 2) * 128), np.arange(offs[5] + j * 256, offs[5] + (j + 1) * 256),
            np.arange(offs[2] + 2 * j * 128, offs[2] + (2 * j + 2) * 128), np.arange(offs[6] + j * 256, offs[6] + (j + 1) * 256),
            np.arange(offs[3] + 2 * j, offs[3] + 2 * j + 2)])
        maps.append({
            "x": np.ascontiguousarray(x[b]),
            "wc": np.ascontiguousarray(w_in[:, cols]),
            "gcol": gcol, "ident": ident,
            "fbias": np.ascontiguousarray(inp["forget_bias"][0][2 * j:2 * j + 2][None]),
            "alibi": np.ascontiguousarray((np.float32(slopes[j]) * rel).astype(np.float32).reshape(128, 2048)),
            "tri": tri, "ones": ones, "lamv": np.ascontiguousarray(lamv.astype(np.float32)),
            "sublng": np.ascontiguousarray(inp["diff_subln_g"]),
        })
    return maps


def build_attn_only(nq=32, do_fox=(0, 1), do_diff=True):
    nc = bass.Bass("TRN2", target_bir_lowering=False)
    fbias = nc.dram_tensor("fbias", [1, 2], F32, kind="ExternalInput").ap()
    alibi = nc.dram_tensor("alibi", [128, 2048], F32, kind="ExternalInput").ap()
    tri = nc.dram_tensor("tri", [128, 128], F32, kind="ExternalInput").ap()
    ones = nc.dram_tensor("ones", [128, 128], F32, kind="ExternalInput").ap()
    lamv = nc.dram_tensor("lamv", [1, 4, 128], F32, kind="ExternalInput").ap()
    sublng = nc.dram_tensor("sublng", [1, 256], F32, kind="ExternalInput").ap()
    qkT = nc.dram_tensor("qkT", [8, 128, S], BF16, kind="ExternalInput").ap()
    vdr = nc.dram_tensor("vdr", [S, 512], BF16, kind="ExternalInput").ap()
    ffd = nc.dram_tensor("ffd", [128, 128], F32, kind="ExternalInput").ap()
    o = nc.dram_tensor("o", [S, 512], BF16, kind="ExternalOutput").ap()
    dbg = nc.dram_tensor("dbg", [128, 2240], F32, kind="ExternalOutput").ap()
    P = Prog(nc)
    phase_attn(P, nc, qkT, vdr, ffd, fbias, alibi, tri, ones, lamv, sublng, o, True, nq=nq, do_fox=do_fox, do_diff=do_diff, dbg=dbg)
    return nc


NT = 2048


def build_l2a(with_router=False):
    nc = bass.Bass("TRN2", target_bir_lowering=False)
    oT = nc.dram_tensor("oT", [16, 128, NT], BF16, kind="ExternalInput").ap()
    xs = nc.dram_tensor("xs", [NT, D], F32, kind="ExternalInput").ap()
    wo = nc.dram_tensor("wo", [D, D], F32, kind="ExternalInput").ap()
    gf = nc.dram_tensor("gf", [1, D], F32, kind="ExternalInput").ap()
    h_o = nc.dram_tensor("h", [NT, D], F32, kind="ExternalOutput").ap()
    hnf_o = nc.dram_tensor("hnf", [NT, D], F32, kind="ExternalOutput").ap()
    hnb_o = nc.dram_tensor("hnb", [NT, D], BF16, kind="ExternalOutput").ap()
    if with_router:
        wr = nc.dram_tensor("wr", [D, 72], F32, kind="ExternalInput").ap()
        br = nc.dram_tensor("br", [1, 72], F32, kind="ExternalInput").ap()
        io8 = nc.dram_tensor("io8", [1, 8], F32, kind="ExternalInput").ap()
        idf = nc.dram_tensor("identf", [128, 128], F32, kind="ExternalInput").ap()
        ri_o = nc.dram_tensor("ri", [NT, 8], F32, kind="ExternalOutput").ap()
    P = Prog(nc)
    P.begin_phase()
    wob = P.sb("wob", [128, 16, D], BF16)
    wst = [P.sb(f"wst{i}", [128, D], F32) for i in range(2)]
    g = P.sb("g", [128, D], F32)
    oTt = [P.sb(f"oTt{i}", [128, 16, 128], BF16) for i in range(2)]
    xt = [P.sb(f"xt{i}", [128, D], F32) for i in range(2)]
    ht = [P.sb(f"ht{i}", [128, D], F32) for i in range(2)]
    hn = [P.sb(f"hn{i}", [128, D], F32) for i in range(2)]
    hb = [P.sb(f"hb{i}", [128, D], BF16) for i in range(2)]
    junk = P.sb("junk", [128, D], BF16)
    ss = [P.sb(f"ss{i}", [128, 1], F32) for i in range(2)]
    nph = 1 if with_router else 2
    ph = P.ps("ph", [128, nph, 4, 512], F32)
    if with_router:
        wrs = P.sb("wrs", [128, 16, 72], F32); brs = P.sb("brs", [128, 72], F32); iota = P.sb("iota", [128, 8], F32)
        identf = P.sb("identf", [128, 128], F32)
        hT32 = P.sb("hT32", [128, 16, 128], F32)
        ptf = P.ps("ptf", [128, 4, 128], F32)
        plog = P.ps("plog", [128, 2, 512], F32)
        P.dma("sp", lambda e: e.dma_start(out=wrs[:], in_=wr.rearrange("(c p) n -> p c n", p=128)), writes=["wrs"])
        P.dma("sp", lambda e: e.dma_start(out=brs[:], in_=br[0:1, :].broadcast_to([128, 72])), writes=["brs"])
        P.dma("sp", lambda e: e.dma_start(out=iota[:], in_=io8[0:1, :].broadcast_to([128, 8])), writes=["iota"])
        P.dma("sp", lambda e: e.dma_start(out=identf[:], in_=idf[:, :]), writes=["identf"])
    P.dma("sp", lambda e: e.dma_start(out=g[:], in_=gf[0:1, :].broadcast_to([128, D])), writes=["g"])
    for c in range(16):
        st = wst[c % 2]
        P.dma("sp", lambda e, st=st, c=c: e.dma_start(out=st[:], in_=wo[c * 128:(c + 1) * 128, :]), writes=[f"wst{c % 2}"])
        P.op("pool", lambda e, st=st, c=c: e.tensor_copy(out=wob[:, c, :], in_=st[:]), reads=[f"wst{c % 2}"], writes=[("wob", c)])
    wall = [("wob", c) for c in range(16)]
    for ti in range(NT // 128):
        b = ti % 2
        P.dma("sp", lambda e, b=b, ti=ti: e.dma_start(out=oTt[b][:], in_=oT[:, :, ti * 128:(ti + 1) * 128].rearrange("c p t -> p c t")), writes=[f"oTt{b}"])
        P.dma("sp", lambda e, b=b, ti=ti: e.dma_start(out=xt[b][:], in_=xs[ti * 128:(ti + 1) * 128, :]), writes=[f"xt{b}"])
        for cc in range(4):
            for c in range(16):
                pbk = b % nph
                P.op("pe", lambda e, b=b, cc=cc, c=c, pbk=pbk: e.matmul(ph[:, pbk, cc, :], lhsT=oTt[b][:, c, :], rhs=wob[:, c, cc * 512:(cc + 1) * 512],
                                                                        start=(c == 0), stop=(c == 15)), reads=wall + [f"oTt{b}"], writes=[("ph", pbk, cc)])
            P.op("dve", lambda e, b=b, cc=cc, pbk=pbk: e.tensor_tensor(out=ht[b][:, cc * 512:(cc + 1) * 512], in0=ph[:, pbk, cc, :], in1=xt[b][:, cc * 512:(cc + 1) * 512], op=ALU.add),
                 reads=[("ph", pbk, cc), f"xt{b}"], writes=[(f"ht{b}", cc)])
        hall = [(f"ht{b}", cc) for cc in range(4)]
        P.dma("pool", lambda e, b=b, ti=ti: e.dma_start(out=h_o[ti * 128:(ti + 1) * 128, :], in_=ht[b][:]), reads=hall, writes=[("h_o", ti)], is_output=True)
        P.op("act", lambda e, b=b: e.activation(out=junk[:], in_=ht[b][:], func=AF.Square, accum_out=ss[b][:]), reads=hall, writes=["junk", f"ss{b}"])
        P.op("act", lambda e, b=b: e.activation(out=ss[b][:], in_=ss[b][:], func=AF.Sqrt, bias=1e-6, scale=1.0 / D), reads=[f"ss{b}"], writes=[f"ss{b}"])
        P.op("dve", lambda e, b=b: e.reciprocal(out=ss[b][:], in_=ss[b][:]), reads=[f"ss{b}"], writes=[f"ss{b}"])
        P.op("dve", lambda e, b=b: e.scalar_tensor_tensor(out=hn[b][:], in0=ht[b][:], scalar=ss[b][:], in1=g[:], op0=ALU.mult, op1=ALU.mult),
             reads=hall + [f"ss{b}", "g"], writes=[f"hn{b}"])
        P.op("act", lambda e, b=b: e.copy(out=hb[b][:], in_=hn[b][:]), reads=[f"hn{b}"], writes=[f"hb{b}"])
        P.dma("pool", lambda e, b=b, ti=ti: e.dma_start(out=hnf_o[ti * 128:(ti + 1) * 128, :], in_=hn[b][:]), reads=[f"hn{b}"], writes=[("hnf_o", ti)], is_output=True)
        P.dma("pool", lambda e, b=b, ti=ti: e.dma_start(out=hnb_o[ti * 128:(ti + 1) * 128, :], in_=hb[b][:]), reads=[f"hb{b}"], writes=[("hnb_o", ti)], is_output=True)
        if with_router:
            for q4 in range(4):
                for i4 in range(4):
                    c = q4 * 4 + i4
                    P.op("pe", lambda e, b=b, c=c, i4=i4: e.transpose(ptf[:, i4, :], hn[b][:, c * 128:(c + 1) * 128], identf[:]),
                         reads=[f"hn{b}", "identf"], writes=["ptf"])
                P.op("act", lambda e, q4=q4: e.copy(out=hT32[:, q4 * 4:(q4 + 1) * 4, :], in_=ptf[:]), reads=["ptf"], writes=[("hT32", q4)])
            for c in range(16):
                P.op("pe", lambda e, b=b, c=c: e.matmul(plog[:, b, 0:72], lhsT=hT32[:, c, :], rhs=wrs[:, c, :], start=(c == 0), stop=(c == 15)),
                     reads=[("hT32", q4) for q4 in range(4)] + ["wrs"], writes=[("plog", b)])
            emit_router(P, ti, b, plog, brs, iota, ri_o)
    P.end_phase(final=True)
    return nc


def emit_router(P, ti, b, plog, brs, iota, ri_o):
    if True:
        T = {}
        def sbt(nm, w):
            T[nm] = P.sb(f"{nm}_{ti}", [128, w], F32)
            return T[nm]
        lg = sbt("lg", 72); goh = sbt("goh", 8); gex = sbt("gex", 8); t8 = sbt("t8", 8); sel = sbt("sel", 8)
        oh1 = sbt("oh1", 8); sel2 = sbt("sel2", 8); oh2 = sbt("oh2", 8); s1 = sbt("s1", 8); ri = sbt("ri", 8)
        rn = lambda k: f"{k}_{ti}"
        def dv(fn, reads, writes):
            P.op("dve", fn, reads=[rn(r) if isinstance(r, str) and not r.startswith("@") else (r[1:] if isinstance(r, str) else r) for r in reads],
                 writes=[rn(w) for w in writes])
        dv(lambda e, b=b, lg=lg: e.tensor_tensor(out=lg[:], in0=plog[:, b, 0:72], in1=brs[:], op=ALU.add), [("plog", b), "@brs"], ["lg"])
        dv(lambda e, lg=lg, s1=s1: e.tensor_reduce(out=s1[:, 0:1], in_=lg[:, 0:8], axis=AX.X, op=ALU.max), ["lg"], ["gm"])
        dv(lambda e, lg=lg, s1=s1, goh=goh: e.tensor_scalar(out=goh[:], in0=lg[:, 0:8], scalar1=s1[:, 0:1], scalar2=None, op0=ALU.is_equal), ["lg", "gm"], ["goh"])
        dv(lambda e, s1=s1: e.tensor_scalar(out=s1[:, 1:2], in0=s1[:, 0:1], scalar1=-1.0, scalar2=None, op0=ALU.mult), ["gm"], ["ngm"])
        P.op("act", lambda e, lg=lg, s1=s1, gex=gex: e.activation(out=gex[:], in_=lg[:, 0:8], func=AF.Exp, bias=s1[:, 1:2], scale=1.0, accum_out=s1[:, 2:3]),
             reads=[rn("lg"), rn("ngm")], writes=[rn("gex"), rn("gsum")])
        dv(lambda e, s1=s1: e.reciprocal(out=s1[:, 3:4], in_=s1[:, 2:3]), ["gsum"], ["gprob"])
        dv(lambda e, goh=goh, t8=t8: e.tensor_tensor(out=t8[:], in0=goh[:], in1=iota[:], op=ALU.mult), ["goh", "@iota"], ["t8"])
        dv(lambda e, t8=t8, ri=ri: e.tensor_reduce(out=ri[:, 0:1], in_=t8[:], axis=AX.X, op=ALU.add), ["t8"], ["ri0"])
        for gi in range(8):
            if gi == 0:
                dv(lambda e, lg=lg, goh=goh, sel=sel: e.tensor_scalar(out=sel[:], in0=lg[:, 8:16], scalar1=goh[:, 0:1], scalar2=None, op0=ALU.mult), ["lg", "goh"], ["sel"])
            else:
                dv(lambda e, lg=lg, goh=goh, sel=sel, gi=gi: e.scalar_tensor_tensor(out=sel[:], in0=lg[:, 8 + gi * 8:16 + gi * 8], scalar=goh[:, gi:gi + 1], in1=sel[:],
                                                                                  op0=ALU.mult, op1=ALU.add), ["lg", "goh", "sel"], ["sel"])
        dv(lambda e, sel=sel, s1=s1: e.tensor_reduce(out=s1[:, 4:5], in_=sel[:], axis=AX.X, op=ALU.max), ["sel"], ["m1"])
        dv(lambda e, sel=sel, s1=s1, oh1=oh1: e.tensor_scalar(out=oh1[:], in0=sel[:], scalar1=s1[:, 4:5], scalar2=None, op0=ALU.is_equal), ["sel", "m1"], ["oh1"])
        dv(lambda e, sel=sel, oh1=oh1, sel2=sel2: e.scalar_tensor_tensor(out=sel2[:], in0=oh1[:], scalar=-1e30, in1=sel[:], op0=ALU.mult, op1=ALU.add), ["sel", "oh1"], ["sel2"])
        dv(lambda e, sel2=sel2, s1=s1: e.tensor_reduce(out=s1[:, 5:6], in_=sel2[:], axis=AX.X, op=ALU.max), ["sel2"], ["m2"])
        dv(lambda e, sel2=sel2, s1=s1, oh2=oh2: e.tensor_scalar(out=oh2[:], in0=sel2[:], scalar1=s1[:, 5:6], scalar2=None, op0=ALU.is_equal), ["sel2", "m2"], ["oh2"])
        dv(lambda e, oh1=oh1, t8=t8: e.tensor_tensor(out=t8[:], in0=oh1[:], in1=iota[:], op=ALU.mult), ["oh1", "@iota", "ri0"], ["t8"])
        dv(lambda e, t8=t8, ri=ri: e.tensor_reduce(out=ri[:, 1:2], in_=t8[:], axis=AX.X, op=ALU.add), ["t8"], ["ri1"])
        dv(lambda e, oh2=oh2, t8=t8: e.tensor_tensor(out=t8[:], in0=oh2[:], in1=iota[:], op=ALU.mult), ["oh2", "@iota", "ri1"], ["t8"])
        dv(lambda e, t8=t8, ri=ri: e.tensor_reduce(out=ri[:, 2:3], in_=t8[:], axis=AX.X, op=ALU.add), ["t8"], ["ri2"])
        dv(lambda e, s1=s1: e.tensor_tensor(out=s1[:, 6:7], in0=s1[:, 5:6], in1=s1[:, 4:5], op=ALU.subtract), ["m1", "m2"], ["dd"])
        P.op("act", lambda e, s1=s1: e.activation(out=s1[:, 6:7], in_=s1[:, 6:7], func=AF.Exp), reads=[rn("dd")], writes=[rn("dd")])
        dv(lambda e, s1=s1: e.tensor_scalar(out=s1[:, 7:8], in0=s1[:, 6:7], scalar1=1.0, scalar2=None, op0=ALU.add), ["dd"], ["w1"])
        dv(lambda e, s1=s1: e.reciprocal(out=s1[:, 7:8], in_=s1[:, 7:8]), ["w1"], ["w1"])
        dv(lambda e, s1=s1, ri=ri: e.tensor_tensor(out=ri[:, 3:4], in0=s1[:, 7:8], in1=s1[:, 3:4], op=ALU.mult), ["w1", "gprob"], ["ri3"])
        dv(lambda e, s1=s1, ri=ri: e.tensor_tensor(out=ri[:, 4:5], in0=ri[:, 3:4], in1=s1[:, 6:7], op=ALU.mult), ["ri3", "dd"], ["ri4"])
        dv(lambda e, ri=ri: e.memset(ri[:, 5:8], 0.0), [], ["ri5"])
        P.dma("sp", lambda e, ri=ri, ti=ti: e.dma_start(out=ri_o[ti * 128:(ti + 1) * 128, :], in_=ri[:]),
              reads=[rn(k) for k in ("ri0", "ri1", "ri2", "ri3", "ri4", "ri5")], writes=[("ri_o", ti)], is_output=True)


def build_l2b():
    nc = bass.Bass("TRN2", target_bir_lowering=False)
    hT = nc.dram_tensor("hT", [16, 128, NT], F32, kind="ExternalInput").ap()
    wr = nc.dram_tensor("wr", [D, 72], F32, kind="ExternalInput").ap()
    br = nc.dram_tensor("br", [1, 72], F32, kind="ExternalInput").ap()
    io8 = nc.dram_tensor("io8", [1, 8], F32, kind="ExternalInput").ap()
    ri_o = nc.dram_tensor("ri", [NT, 8], F32, kind="ExternalOutput").ap()
    P = Prog(nc)
    P.begin_phase()
    wrs = P.sb("wrs", [128, 16, 72], F32)
    brs = P.sb("brs", [128, 72], F32)
    iota = P.sb("iota", [128, 8], F32)
    hTt = [P.sb(f"hTt{i}", [128, 16, 128], F32) for i in range(2)]
    plog = P.ps("plog", [128, 2, 512], F32)
    P.dma("sp", lambda e: e.dma_start(out=wrs[:], in_=wr.rearrange("(c p) n -> p c n", p=128)), writes=["wrs"])
    P.dma("sp", lambda e: e.dma_start(out=brs[:], in_=br[0:1, :].broadcast_to([128, 72])), writes=["brs"])
    P.dma("sp", lambda e: e.dma_start(out=iota[:], in_=io8[0:1, :].broadcast_to([128, 8])), writes=["iota"])
    names = ["lg", "goh", "gex", "t8", "sel", "oh1", "sel2", "oh2"]
    for ti in range(NT // 128):
        b = ti % 2
        P.dma("sp", lambda e, b=b, ti=ti: e.dma_start(out=hTt[b][:], in_=hT[:, :, ti * 128:(ti + 1) * 128].rearrange("c p t -> p c t")), writes=[f"hTt{b}"])
        for c in range(16):
            P.op("pe", lambda e, b=b, c=c: e.matmul(plog[:, b, 0:72], lhsT=hTt[b][:, c, :], rhs=wrs[:, c, :], start=(c == 0), stop=(c == 15)),
                 reads=[f"hTt{b}", "wrs"], writes=[("plog", b)])
        emit_router(P, ti, b, plog, brs, iota, ri_o)
    P.end_phase(final=True)
    return nc


def build_l3(cap):
    ncb = cap // 128
    nc = bass.Bass("TRN2", target_bir_lowering=False)
    XT = nc.dram_tensor("XT", [8, 16, 128, cap], BF16, kind="ExternalInput").ap()
    gcol = nc.dram_tensor("gcol", [128, 8 * ncb], F32, kind="ExternalInput").ap()
    wg = nc.dram_tensor("wg", [8, D, 1024], F32, kind="ExternalInput").ap()
    wu = nc.dram_tensor("wu", [8, D, 1024], F32, kind="ExternalInput").ap()
    wd = nc.dram_tensor("wd", [8, 1024, D], F32, kind="ExternalInput").ap()
    Y = nc.dram_tensor("Y", [8, cap, D], F32, kind="ExternalOutput").ap()
    P = Prog(nc)
    P.begin_phase()
    wbuf = [P.sb(f"wbuf{i}", [128, 16384], BF16) for i in range(4)]
    nxb = 2 if cap <= 640 else 1
    xT = [P.sb(f"xT{i}", [128, 16, cap], BF16) for i in range(nxb)]
    gc = P.sb("gc", [128, 8 * ncb], F32)
    nch = (cap + 511) // 512
    cw = ((ncb + nch - 1) // nch) * 128
    chunks = [(r0, min(cw, cap - r0)) for r0 in range(0, cap, cw)]
    pbi = [0]
    sg = [P.sb(f"sg{i}", [128, cw], F32) for i in range(2)]
    hdn = P.sb("hdn", [128, 8, cap], BF16)
    ys = [P.sb(f"ys{i}", [128, D], F32) for i in range(2)]
    pg = P.ps("pg", [128, 2, 512], F32)
    pu = P.ps("pu", [128, 2, 512], F32)
    py = P.ps("py", [128, 2, 512], F32)
    P.dma("sp", lambda e: e.dma_start(out=gc[:], in_=gcol[:, :]), writes=["gc"])
    wi = 0
    yi = 0
    for ex in range(8):
        xb = ex % nxb
        P.dma("sp", lambda e, xb=xb, ex=ex: e.dma_start(out=xT[xb][:], in_=XT[ex].rearrange("c p r -> p c r")), writes=[f"xT{xb}"])
        bufs = []
        for (w, kk) in ((wg, 16), (wu, 16), (wd, 8)):
            bi = wi % 4; wi += 1
            for half in range(2):
                k2 = kk // 2
                def ld(e, bi=bi, w=w, ex=ex, kk=kk, half=half, k2=k2):
                    ncol = 16384 // kk
                    dst = wbuf[bi][:].rearrange("p (c f) -> p c f", c=kk)[:, half * k2:(half + 1) * k2, :]
                    src = w[ex].rearrange("(c p) f -> p c f", p=128)[:, half * k2:(half + 1) * k2, :]
                    return e.dma_start(out=dst, in_=src)
                P.dma("pool", ld, writes=[(f"wbuf{bi}", half)])
            bufs.append(bi)
        bg, bu, bd = bufs
        wgv = wbuf[bg][:].rearrange("p (c f) -> p c f", c=16)
        wuv = wbuf[bu][:].rearrange("p (c f) -> p c f", c=16)
        wdv = wbuf[bd][:].rearrange("p (c f) -> p c f", c=8)
        for (r0, rw) in chunks:
            for f in range(8):
                pb = pbi[0] % 2; pbi[0] += 1
                for c in range(16):
                    P.op("pe", lambda e, pb=pb, c=c, f=f, wgv=wgv, xb=xb, r0=r0, rw=rw: e.matmul(pg[:, pb, 0:rw], lhsT=wgv[:, c, f * 128:(f + 1) * 128],
                                                                                               rhs=xT[xb][:, c, r0:r0 + rw], start=(c == 0), stop=(c == 15)),
                         reads=[(f"wbuf{bg}", 0), (f"wbuf{bg}", 1), f"xT{xb}"], writes=[("pg", pb)])
                for c in range(16):
                    P.op("pe", lambda e, pb=pb, c=c, f=f, wuv=wuv, xb=xb, r0=r0, rw=rw: e.matmul(pu[:, pb, 0:rw], lhsT=wuv[:, c, f * 128:(f + 1) * 128],
                                                                                               rhs=xT[xb][:, c, r0:r0 + rw], start=(c == 0), stop=(c == 15)),
                         reads=[(f"wbuf{bu}", 0), (f"wbuf{bu}", 1), f"xT{xb}"], writes=[("pu", pb)])
                P.op("act", lambda e, pb=pb, rw=rw: e.activation(out=sg[pb][:, 0:rw], in_=pg[:, pb, 0:rw], func=AF.Silu), reads=[("pg", pb)], writes=[f"sg{pb}"])
                P.op("dve", lambda e, pb=pb, f=f, r0=r0, rw=rw: e.tensor_tensor(out=hdn[:, f, r0:r0 + rw], in0=sg[pb][:, 0:rw], in1=pu[:, pb, 0:rw], op=ALU.mult),
                     reads=[f"sg{pb}", ("pu", pb)], writes=[("hdn", f, r0)])
        hall = [("hdn", f, r0) for f in range(8) for (r0, rw) in chunks]
        for rb in range(ncb):
            yb = yi % 2; yi += 1
            for cc in range(4):
                pb = cc % 2
                for f in range(8):
                    P.op("pe", lambda e, pb=pb, f=f, rb=rb, cc=cc, wdv=wdv: e.matmul(py[:, pb, :], lhsT=hdn[:, f, rb * 128:(rb + 1) * 128],
                                                                                     rhs=wdv[:, f, cc * 512:(cc + 1) * 512], start=(f == 0), stop=(f == 7)),
                         reads=hall + [(f"wbuf{bd}", 0), (f"wbuf{bd}", 1)], writes=[("py", pb)])
                gi = ex * ncb + rb
                P.op("dve", lambda e, pb=pb, yb=yb, cc=cc, gi=gi: e.tensor_scalar(out=ys[yb][:, cc * 512:(cc + 1) * 512], in0=py[:, pb, :], scalar1=gc[:, gi:gi + 1],
                                                                                  scalar2=None, op0=ALU.mult), reads=[("py", pb), "gc"], writes=[(f"ys{yb}", cc)])
            P.dma("sp", lambda e, yb=yb, ex=ex, rb=rb: e.dma_start(out=Y[ex, rb * 128:(rb + 1) * 128, :], in_=ys[yb][:]),
                  reads=[(f"ys{yb}", cc) for cc in range(4)], writes=[("Y", ex, rb)], is_output=True)
    P.end_phase(final=True)
    return nc


def build_l4():
    nc = bass.Bass("TRN2", target_bir_lowering=False)
    h = nc.dram_tensor("h", [NT, D], F32, kind="ExternalInput").ap()
    y1 = nc.dram_tensor("y1", [NT, D], F32, kind="ExternalInput").ap()
    y2 = nc.dram_tensor("y2", [NT, D], F32, kind="ExternalInput").ap()
    gf = nc.dram_tensor("gf", [1, D], F32, kind="ExternalInput").ap()
    out = nc.dram_tensor("out", [NT, D], F32, kind="ExternalOutput").ap()
    P = Prog(nc)
    P.begin_phase()
    g = P.sb("g", [128, D], F32)
    a = [P.sb(f"a{i}", [128, D], F32) for i in range(2)]
    b1 = [P.sb(f"b1{i}", [128, D], F32) for i in range(2)]
    b2 = [P.sb(f"b2{i}", [128, D], F32) for i in range(2)]
    ot = [P.sb(f"ot{i}", [128, D], F32) for i in range(2)]
    junk = P.sb("junk", [128, D], BF16)
    ss = [P.sb(f"ss{i}", [128, 1], F32) for i in range(2)]
    P.dma("sp", lambda e: e.dma_start(out=g[:], in_=gf[0:1, :].broadcast_to([128, D])), writes=["g"])
    for ti in range(NT // 128):
        b = ti % 2
        sl = slice(ti * 128, (ti + 1) * 128)
        P.dma("sp", lambda e, b=b, sl=sl: e.dma_start(out=a[b][:], in_=h[sl, :]), writes=[f"a{b}"])
        P.dma("sp", lambda e, b=b, sl=sl: e.dma_start(out=b1[b][:], in_=y1[sl, :]), writes=[f"b1{b}"])
        P.dma("sp", lambda e, b=b, sl=sl: e.dma_start(out=b2[b][:], in_=y2[sl, :]), writes=[f"b2{b}"])
        P.op("pool", lambda e, b=b: e.tensor_tensor(out=b1[b][:], in0=b1[b][:], in1=b2[b][:], op=ALU.add), reads=[f"b1{b}", f"b2{b}"], writes=[f"b1{b}"])
        P.op("dve", lambda e, b=b: e.tensor_tensor(out=a[b][:], in0=a[b][:], in1=b1[b][:], op=ALU.add), reads=[f"a{b}", f"b1{b}"], writes=[f"a{b}"])
        P.op("act", lambda e, b=b: e.activation(out=junk[:], in_=a[b][:], func=AF.Square, accum_out=ss[b][:]), reads=[f"a{b}"], writes=["junk", f"ss{b}"])
        P.op("act", lambda e, b=b: e.activation(out=ss[b][:], in_=ss[b][:], func=AF.Sqrt, bias=1e-6, scale=1.0 / D), reads=[f"ss{b}"], writes=[f"ss{b}"])
        P.op("dve", lambda e, b=b: e.reciprocal(out=ss[b][:], in_=ss[b][:]), reads=[f"ss{b}"], writes=[f"ss{b}"])
        P.op("dve", lambda e, b=b: e.scalar_tensor_tensor(out=ot[b][:], in0=a[b][:], scalar=ss[b][:], in1=g[:], op0=ALU.mult, op1=ALU.mult),
             reads=[f"a{b}", f"ss{b}", "g"], writes=[f"ot{b}"])
        P.dma("pool", lambda e, b=b, sl=sl: e.dma_start(out=out[sl, :], in_=ot[b][:]), reads=[f"ot{b}"], writes=[("out", ti)], is_output=True)
    P.end_phase(final=True)
    return nc


def _run(nc, maps):
    res = run_bass_kernel_spmd(nc, maps, core_ids=list(range(NCORES)))
    return res.results


def kernel(**inp):
    inp = {k: np.asarray(v) for k, v in inp.items()}
    N = NB * S
    r1 = _run(build_l1(), l1_inputs(inp))
    O = np.empty((NB, S, D), dtype=NPBF)
    for c in range(NCORES):
        b, j = c // 4, c % 4
        o = np.asarray(r1[c]["o"])
        if o.dtype != NPBF:
            o = o.view(NPBF)
        O[b, :, 2 * j * 128:(2 * j + 2) * 128] = o[:, 0:256]
        O[b, :, 1024 + j * 256:1024 + (j + 1) * 256] = o[:, 256:512]
    Of = O.reshape(N, D)
    xf = inp["x"].reshape(N, D)
    wr = np.ascontiguousarray(np.concatenate([inp["router_group_w"][0], inp["router_expert_w"][0]], 1))
    br = np.ascontiguousarray(np.concatenate([inp["router_group_b"][0], inp["router_expert_b"][0]])[None])
    io8 = np.arange(8, dtype=np.float32)[None]
    identf = np.eye(128, dtype=np.float32)
    maps = []
    for c in range(NCORES):
        sl = slice(c * NT, (c + 1) * NT)
        maps.append({"oT": np.ascontiguousarray(Of[sl].T).reshape(16, 128, NT), "xs": np.ascontiguousarray(xf[sl]),
                     "wo": np.ascontiguousarray(inp["w_out"][0]), "gf": np.ascontiguousarray(inp["ffn_norm_g"]),
                     "wr": wr, "br": br, "io8": io8, "identf": identf})
    r2 = _run(build_l2a(with_router=True), maps)
    h = np.concatenate([np.asarray(r["h"]) for r in r2], 0)
    hnb = np.concatenate([np.asarray(r["hnb"]) for r in r2], 0)
    if hnb.dtype != NPBF:
        hnb = hnb.view(NPBF)
    ri = np.concatenate([np.asarray(r["ri"]) for r in r2], 0)
    gidx = np.rint(ri[:, 0]).astype(np.int64)
    eloc = np.rint(ri[:, 1:3]).astype(np.int64)
    gates = ri[:, 3:5]
    rows = [[[] for _ in range(8)] for _ in range(8)]
    slot = np.zeros((N, 2), np.int64)
    for k in range(2):
        for g in range(8):
            for e in range(8):
                idx = np.nonzero((gidx == g) & (eloc[:, k] == e))[0]
                base = len(rows[g][e])
                slot[idx, k] = base + np.arange(len(idx))
                rows[g][e].extend([(int(t), k) for t in idx])
    mx = max(len(rows[g][e]) for g in range(8) for e in range(8))
    cap = max(128, ((mx + 127) // 128) * 128)
    ncb = cap // 128
    maps = []
    for g in range(8):
        XT = np.zeros((8, cap, D), dtype=NPBF)
        gc = np.zeros((8, cap), np.float32)
        for e in range(8):
            if rows[g][e]:
                t = np.array([a for a, _ in rows[g][e]]); kk = np.array([b for _, b in rows[g][e]])
                XT[e, :len(t)] = hnb[t]
                gc[e, :len(t)] = gates[t, kk]
        XTt = np.ascontiguousarray(XT.transpose(0, 2, 1)).reshape(8, 16, 128, cap)
        gcol = np.ascontiguousarray(gc.reshape(8, ncb, 128).transpose(2, 0, 1).reshape(128, 8 * ncb))
        maps.append({"XT": XTt, "gcol": gcol,
                     "wg": np.ascontiguousarray(inp["w_gate"][0, g * 8:(g + 1) * 8]), "wu": np.ascontiguousarray(inp["w_up"][0, g * 8:(g + 1) * 8]),
                     "wd": np.ascontiguousarray(inp["w_down"][0, g * 8:(g + 1) * 8])})
    r4 = _run(build_l3(cap), maps)
    Yall = np.stack([np.asarray(r["Y"]) for r in r4], 0)
    y1 = Yall[gidx, eloc[:, 0], slot[:, 0]]
    y2 = Yall[gidx, eloc[:, 1], slot[:, 1]]
    maps = [{"h": np.ascontiguousarray(h[c * NT:(c + 1) * NT]), "y1": np.ascontiguousarray(y1[c * NT:(c + 1) * NT]),
             "y2": np.ascontiguousarray(y2[c * NT:(c + 1) * NT]), "gf": np.ascontiguousarray(inp["final_norm_g"][None])} for c in range(NCORES)]
    r5 = _run(build_l4(), maps)
    out = np.concatenate([np.asarray(r["out"]) for r in r5], 0).reshape(NB, S, D).astype(np.float32)
    return out
```

```python
from contextlib import ExitStack
import numpy as np
import ml_dtypes
import concourse.bass as bass
import concourse.mybir as mybir
from concourse.bass_utils import run_bass_kernel_spmd

F32 = mybir.dt.float32
BF16 = mybir.dt.bfloat16
I32 = mybir.dt.int32
AF = mybir.ActivationFunctionType
ALU = mybir.AluOpType
AX = mybir.AxisListType
NPBF = ml_dtypes.bfloat16

D = 2048
S = 8192
NB = 2
NCORES = 8
HD = 128
NWC = 1538
SCALE = HD ** -0.5
LAM_INIT = 0.2
ENGS = ("pe", "act", "dve", "pool", "sp")
NDMASEM = 8


class Prog:
    def __init__(self, nc):
        self.nc = nc
        self.es = ExitStack()
        self.ops = {e: [] for e in ENGS}
        self.cnt = {e: 0 for e in ENGS}
        self.sem = {e: self.es.enter_context(nc.semaphore("s_" + e)) for e in ENGS}
        self.dsem = {q: [self.es.enter_context(nc.semaphore(f"d_{q}{i}")) for i in range(NDMASEM)]
                     for q in ("sp", "pool")}
        self.dcnt = {"sp": 0, "pool": 0}
        self.known = {e: {} for e in ENGS}
        self.lastw = {}
        self.readers = {}
        self.final_waits = []
        self.phase_es = None

    def begin_phase(self):
        self.phase_es = ExitStack()

    def sb(self, name, shape, dt):
        return self.phase_es.enter_context(self.nc.sbuf_tensor("sb_" + name, list(shape), dt))

    def ps(self, name, shape, dt):
        return self.phase_es.enter_context(self.nc.psum_tensor("ps_" + name, list(shape), dt))

    def _deps(self, eng, reads, writes):
        deps = set()
        for r in reads:
            w = self.lastw.get(r)
            if w is not None:
                deps.add(w)
        for w_ in writes:
            w = self.lastw.get(w_)
            if w is not None:
                deps.add(w)
            for rd in self.readers.get(w_, ()):
                deps.add(rd)
        best = {}
        for d in deps:
            if d[0] == "pe" and eng == "pe":
                continue
            if self.known[eng].get(d[0], 0) >= d[1]:
                continue
            if best.get(d[0], 0) < d[1]:
                best[d[0]] = d[1]
        for s, v in best.items():
            self.known[eng][s] = v
        return list(best.items())

    def _mark(self, token, reads, writes):
        for r in reads:
            self.readers.setdefault(r, []).append(token)
        for w in writes:
            self.lastw[w] = token
            self.readers[w] = []

    def op(self, eng, fn, reads=(), writes=(), after=()):
        waits = self._deps(eng, reads, writes)
        for (src, v) in after:
            if self.known[eng].get(src, 0) < v:
                waits.append((src, v))
                self.known[eng][src] = v
        self.cnt[eng] += 1
        token = (eng, self.cnt[eng])
        self.ops[eng].append((waits, fn, ("eng", eng)))
        self._mark(token, reads, writes)
        return token

    def dma(self, q, fn, reads=(), writes=(), is_output=False, inc=16):
        waits = self._deps(q, reads, writes)
        i = self.dcnt[q]
        self.dcnt[q] += 1
        slot = i % NDMASEM
        val = 16 * (i // NDMASEM + 1)
        src = ("d", q, slot)
        if i >= NDMASEM and self.known[q].get(src, 0) < val - 16:
            waits.append((src, val - 16))
            self.known[q][src] = val - 16
        token = (src, val)
        self.ops[q].append((waits, fn, ("dma", q, slot, inc)))
        self._mark(token, reads, writes)
        if is_output:
            self.final_waits.append(token)
        return token

    def _semof(self, src):
        if isinstance(src, tuple):
            return self.csem[src[1]] if src[0] == "c" else self.dsem[src[1]][src[2]]
        return self.sem[src]

    def collective(self, fn, writes=(), inc=16):
        if not hasattr(self, "csem"):
            self.csem = []
        sem = self.es.enter_context(self.nc.semaphore(f"cc{len(self.csem)}"))
        self.csem.append(sem)
        k = len(self.csem) - 1
        fn(self.nc.gpsimd).then_inc(sem, inc)
        self.nc.gpsimd.wait_ge(sem, inc)
        token = (("c", k), inc)
        self._mark(token, (), writes)
        return token

    def end_phase(self, final=False):
        allw = [(e, self.cnt[e]) for e in ENGS if self.cnt[e] > 0]
        for q in ("sp", "pool"):
            n = self.dcnt[q]
            for slot in range(min(n, NDMASEM)):
                last_i = ((n - 1 - slot) // NDMASEM) * NDMASEM + slot
                allw.append((("d", q, slot), 16 * (last_i // NDMASEM + 1)))
        for e in ENGS:
            w = [(s, v) for (s, v) in allw if self.known[e].get(s, 0) < v]
            for s, v in w:
                self.known[e][s] = v
            self.ops[e].append((w, None, None))
        nc = self.nc
        with nc.Block() as block:
            def run(eng_name):
                def body(e):
                    for waits, fn, inc in self.ops[eng_name]:
                        for (src, val) in waits:
                            e.wait_ge(self._semof(src), val)
                        if fn is None:
                            continue
                        ins = fn(e)
                        if inc[0] == "eng":
                            ins.then_inc(self.sem[inc[1]], 1)
                        else:
                            ins.then_inc(self.dsem[inc[1]][inc[2]], inc[3])
                return body
            block.tensor(run("pe"))
            block.scalar(run("act"))
            block.vector(run("dve"))
            block.gpsimd(run("pool"))
            block.sync(run("sp"))
        self.ops = {e: [] for e in ENGS}
        self.phase_es.close()
        self.phase_es = None
        if final:
            self.es.close()


def phase_proj(P, nc, x, wc, gcol, ident_d, qkT, vdr, ffd):
    P.begin_phase()
    wb = P.sb("wb", [128, 16, NWC], BF16)
    wst = [P.sb(f"wst{i}", [128, NWC], F32) for i in range(2)]
    gc = P.sb("gc", [128, 16], F32)
    ident = P.sb("ident", [128, 128], BF16)
    xt = [P.sb(f"xt{i}", [128, D], F32) for i in range(3)]
    junk = P.sb("junk", [128, D], BF16)
    ab = [P.sb(f"ab{i}", [128, D], BF16) for i in range(2)]
    ss = [P.sb(f"ss{i}", [128, 1], F32) for i in range(2)]
    rstd = [P.sb(f"rstd{i}", [128, 1], F32) for i in range(2)]
    aT = [P.sb(f"aT{i}", [128, 16, 512], BF16) for i in range(2)]
    qkst = [P.sb(f"qkst{i}", [128, 8, 512], BF16) for i in range(2)]
    vst = [P.sb(f"vst{i}", [128, 512], BF16) for i in range(2)]
    ffs = P.sb("ffs", [128, 128], F32)
    ptr = P.ps("ptr", [128, D], BF16)
    pqk = P.ps("pqk", [128, 2, 512], F32)
    pv = P.ps("pv", [128, 2, 512], F32)
    pf = P.ps("pf", [128, 2, 512], F32)

    P.dma("sp", lambda e: e.dma_start(out=gc[:], in_=gcol[:, :]), writes=["gc"])
    P.dma("sp", lambda e: e.dma_start(out=ident[:], in_=ident_d[:, :]), writes=["ident"])
    for c in range(16):
        st = wst[c % 2]
        P.dma("sp", lambda e, st=st, c=c: e.dma_start(out=st[:], in_=wc[c * 128:(c + 1) * 128, :]),
              writes=[f"wst{c % 2}"])
        P.op("pool", lambda e, st=st, c=c: e.tensor_scalar(out=wb[:, c, :], in0=st[:], scalar1=gc[:, c:c + 1],
                                                          scalar2=None, op0=ALU.mult),
             reads=[f"wst{c % 2}", "gc"], writes=[("wb", c)])
    wb_all = [("wb", c) for c in range(16)]

    tile_i = 0
    for gi in range(16):
        aTg = aT[gi % 2]
        aTn = f"aT{gi % 2}"
        for tt in range(4):
            ti = gi * 4 + tt
            xs = xt[ti % 3]; xn = f"xt{ti % 3}"
            abt = ab[ti % 2]; abn = f"ab{ti % 2}"
            sst = ss[ti % 2]; ssn = f"ss{ti % 2}"
            rs = rstd[ti % 2]; rsn = f"rstd{ti % 2}"
            P.dma("sp", lambda e, xs=xs, ti=ti: e.dma_start(out=xs[:], in_=x[ti * 128:(ti + 1) * 128, :]), writes=[xn])
            P.op("act", lambda e, xs=xs, sst=sst: e.activation(out=junk[:], in_=xs[:], func=AF.Square, accum_out=sst[:]),
                 reads=[xn], writes=["junk", ssn])
            P.op("act", lambda e, sst=sst: e.activation(out=sst[:], in_=sst[:], func=AF.Sqrt, bias=1e-6, scale=1.0 / D),
                 reads=[ssn], writes=[ssn])
            P.op("dve", lambda e, sst=sst, rs=rs: e.reciprocal(out=rs[:], in_=sst[:]), reads=[ssn], writes=[rsn])
            P.op("dve", lambda e, xs=xs, rs=rs, abt=abt: e.tensor_scalar(out=abt[:], in0=xs[:], scalar1=rs[:], scalar2=None, op0=ALU.mult),
                 reads=[xn, rsn], writes=[abn])
            for c in range(16):
                P.op("pe", lambda e, c=c, abt=abt: e.transpose(ptr[:, c * 128:(c + 1) * 128], abt[:, c * 128:(c + 1) * 128], ident[:]),
                     reads=[abn, "ident"], writes=[("ptr", c // 8)])
            for hh in range(2):
                eng = "act" if hh == 0 else "dve"
                def cp(e, hh=hh, aTg=aTg, tt=tt, eng=eng):
                    o = aTg[:, hh * 8:(hh + 1) * 8, tt * 128:(tt + 1) * 128]
                    i = ptr[:, hh * 1024:(hh + 1) * 1024].rearrange("p (c t) -> p c t", c=8)
                    if eng == "act":
                        return e.copy(out=o, in_=i)
                    return e.tensor_copy(out=o, in_=i)
                P.op(eng, cp, reads=[("ptr", hh)], writes=[(aTn, tt)])
        aT_all = [(aTn, tt) for tt in range(4)]
        qs = qkst[gi % 2]; qsn = f"qkst{gi % 2}"
        for cb in range(8):
            bank = cb % 2
            for c in range(16):
                P.op("pe", lambda e, cb=cb, c=c, bank=bank, aTg=aTg: e.matmul(pqk[:, bank, :], lhsT=wb[:, c, cb * 128:(cb + 1) * 128],
                                                                              rhs=aTg[:, c, :], start=(c == 0), stop=(c == 15)),
                     reads=wb_all + aT_all, writes=[("pqk", bank)])
            eng = "act" if cb % 2 == 0 else "dve"
            def ev(e, cb=cb, bank=bank, qs=qs, eng=eng):
                if eng == "act":
                    return e.copy(out=qs[:, cb, :], in_=pqk[:, bank, :])
                return e.tensor_copy(out=qs[:, cb, :], in_=pqk[:, bank, :])
            P.op(eng, ev, reads=[("pqk", bank)], writes=[(qsn, cb)])
        P.dma("pool", lambda e, qs=qs, gi=gi: e.dma_start(out=qkT[:, :, gi * 512:(gi + 1) * 512].rearrange("c p t -> p c t"), in_=qs[:]),
              reads=[(qsn, cb) for cb in range(8)], writes=[("qkT", gi)])
        for tt in range(4):
            ti = gi * 4 + tt
            bank = ti % 2
            vs = vst[ti % 2]; vsn = f"vst{ti % 2}"
            for c in range(16):
                P.op("pe", lambda e, c=c, bank=bank, tt=tt, aTg=aTg: e.matmul(pv[:, bank, :], lhsT=aTg[:, c, tt * 128:(tt + 1) * 128],
                                                                              rhs=wb[:, c, 1024:1536], start=(c == 0), stop=(c == 15)),
                     reads=wb_all + aT_all, writes=[("pv", bank)])
            for c in range(16):
                P.op("pe", lambda e, c=c, bank=bank, tt=tt, aTg=aTg: e.matmul(pf[:, bank, 0:2], lhsT=aTg[:, c, tt * 128:(tt + 1) * 128],
                                                                              rhs=wb[:, c, 1536:1538], start=(c == 0), stop=(c == 15)),
                     reads=wb_all + aT_all, writes=[("pf", bank)])
            eng = "act" if tt % 2 == 0 else "dve"
            def evv(e, bank=bank, vs=vs, eng=eng):
                if eng == "act":
                    return e.copy(out=vs[:], in_=pv[:, bank, :])
                return e.tensor_copy(out=vs[:], in_=pv[:, bank, :])
            P.op(eng, evv, reads=[("pv", bank)], writes=[vsn])
            P.op("dve", lambda e, bank=bank, ti=ti: e.tensor_copy(out=ffs[:, ti * 2:ti * 2 + 2], in_=pf[:, bank, 0:2]),
                 reads=[("pf", bank)], writes=[("ffs", ti)])
            P.dma("pool", lambda e, vs=vs, ti=ti: e.dma_start(out=vdr[ti * 128:(ti + 1) * 128, :], in_=vs[:]),
                  reads=[vsn], writes=[("vdr", ti)])
    P.dma("pool", lambda e: e.dma_start(out=ffd[:, :], in_=ffs[:]), reads=[("ffs", ti) for ti in range(64)], writes=["ffd"])
    P.end_phase()


def phase_attn(P, nc, qkT, vdr, ffd, fbias, alibi_d, tri_d, ones_d, lamv, sublng, o_out, out_is_final, nq=32, do_fox=(0, 1), do_diff=True, dbg=None):
    P.begin_phase()
    KT = P.sb("KT", [128, 4, S], BF16)
    Vf = [P.sb(f"Vf{h}", [128, 64, 129], BF16) for h in range(2)]
    Vd = P.sb("Vd", [128, 64, 257], BF16)
    G = P.sb("G", [128, 32, 64], F32)
    tri = P.sb("tri", [128, 128], F32)
    trib = P.sb("trib", [128, 128], BF16)
    onesf = P.sb("onesf", [128, 128], F32)
    ff = P.sb("ff", [128, 128], F32)
    fb = P.sb("fb", [128, 2], F32)
    lf = P.sb("lf", [128, 64], F32)
    ccol = P.sb("ccol", [128, 64], F32)
    sc = [P.sb(f"sc{i}", [128, 64], F32) for i in range(2)]
    ex = P.sb("ex", [128, 64], F32)
    lv = P.sb("lv", [128, 4, 128], F32)
    lsc = P.sb("lsc", [128, 128], F32)
    l1 = P.sb("l1", [128, 1], F32)
    l2 = P.sb("l2", [128, 1], F32)
    neglam = P.sb("neglam", [128, 1], F32)
    gs = P.sb("gs", [128, 256], F32)
    qt_ = [P.sb(f"qt{i}", [128, 256], BF16) for i in range(4)]
    pt = [P.sb(f"pt{i}", [128, 256], BF16) for i in range(4)]
    ost = [P.sb(f"ost{i}", [128, 2, 256], BF16) for i in range(2)]
    rec = [P.sb(f"rec{i}", [128, 1], F32) for i in range(4)]
    t0 = [P.sb(f"t0{i}", [128, 256], F32) for i in range(2)]
    t1 = [P.sb(f"t1{i}", [128, 256], F32) for i in range(2)]
    junk2 = P.sb("junk2", [128, 256], F32)
    ssd = [P.sb(f"ssd{i}", [128, 1], F32) for i in range(2)]
    ps_s = P.ps("ps_s", [128, 3, 512], F32)
    po = P.ps("po", [128, 4, 512], F32)
    pc = P.ps("pc", [128, 2, 64], F32)

    for i in range(4):
        P.dma("sp", lambda e, i=i: e.dma_start(out=KT[:, i, :], in_=qkT[4 + i, :, :]), writes=[("KT", i)])
    vview = vdr.rearrange("(k p) c -> p k c", p=128)
    for h in range(2):
        for kq in range(4):
            P.dma("sp", lambda e, h=h, kq=kq: e.dma_start(out=Vf[h][:, kq * 16:(kq + 1) * 16, 0:128],
                                                         in_=vview[:, kq * 16:(kq + 1) * 16, h * 128:(h + 1) * 128]), writes=[(f"Vf{h}", kq)])
        P.op("pool", lambda e, h=h: e.memset(Vf[h][:, :, 128:129], 1.0), reads=[(f"Vf{h}", kq) for kq in range(4)], writes=[f"Vf{h}o"])
    for kq in range(4):
        P.dma("sp", lambda e, kq=kq: e.dma_start(out=Vd[:, kq * 16:(kq + 1) * 16, 0:256], in_=vview[:, kq * 16:(kq + 1) * 16, 256:512]), writes=[("Vd", kq)])
    P.op("pool", lambda e: e.memset(Vd[:, :, 256:257], 1.0), reads=[("Vd", kq) for kq in range(4)], writes=["Vdo"])
    P.dma("sp", lambda e: e.dma_start(out=tri[:], in_=tri_d[:, :]), writes=["tri"])
    P.dma("sp", lambda e: e.dma_start(out=onesf[:], in_=ones_d[:, :]), writes=["onesf"])
    P.dma("sp", lambda e: e.dma_start(out=ff[:], in_=ffd[:, :]), writes=["ff"])
    P.dma("sp", lambda e: e.dma_start(out=fb[:], in_=fbias[0:1, :].broadcast_to([128, 2])), writes=["fb"])
    P.dma("sp", lambda e: e.dma_start(out=lv[:], in_=lamv[0:1, :, :].broadcast_to([128, 4, 128])), writes=["lv"])
    P.dma("sp", lambda e: e.dma_start(out=gs[:], in_=sublng[0:1, :].broadcast_to([128, 256])), writes=["gs"])
    P.op("dve", lambda e: e.tensor_copy(out=trib[:], in_=tri[:]), reads=["tri"], writes=["trib"])
    P.op("dve", lambda e: e.tensor_scalar(out=fb[:], in0=fb[:], scalar1=-1.0, scalar2=None, op0=ALU.mult), reads=["fb"], writes=["fb"])
    P.op("dve", lambda e: e.tensor_tensor(out=lsc[:], in0=lv[:, 0, :], in1=lv[:, 1, :], op=ALU.mult), reads=["lv"], writes=["lsc"])
    P.op("dve", lambda e: e.tensor_reduce(out=l1[:], in_=lsc[:], axis=AX.X, op=ALU.add), reads=["lsc"], writes=["l1"])
    P.op("dve", lambda e: e.tensor_tensor(out=lsc[:], in0=lv[:, 2, :], in1=lv[:, 3, :], op=ALU.mult), reads=["lv", "l1"], writes=["lsc"])
    P.op("dve", lambda e: e.tensor_reduce(out=l2[:], in_=lsc[:], axis=AX.X, op=ALU.add), reads=["lsc"], writes=["l2"])
    P.op("act", lambda e: e.activation(out=l1[:], in_=l1[:], func=AF.Exp), reads=["l1"], writes=["l1"])
    P.op("act", lambda e: e.activation(out=l2[:], in_=l2[:], func=AF.Exp), reads=["l2"], writes=["l2"])
    P.op("dve", lambda e: e.tensor_tensor(out=neglam[:], in0=l2[:], in1=l1[:], op=ALU.subtract), reads=["l1", "l2"], writes=["neglam"])
    P.op("dve", lambda e: e.tensor_scalar(out=neglam[:], in0=neglam[:], scalar1=-LAM_INIT, scalar2=None, op0=ALU.add), reads=["neglam"], writes=["neglam"])
    P.op("dve", lambda e: e.tensor_scalar(out=gs[:], in0=gs[:], scalar1=1.0 - LAM_INIT, scalar2=None, op0=ALU.mult), reads=["gs"], writes=["gs"])

    state = {"q": 0, "p": 0, "s": 0, "o": 0, "r": 0}

    def fox_table(h):
        P.op("act", lambda e: e.activation(out=lf[:], in_=ff[:].rearrange("p (k h) -> p k h", h=2)[:, :, h], func=AF.Exp,
                                           bias=fb[:, h:h + 1], scale=-1.0), reads=["ff", "fb"], writes=["lf"])
        P.op("act", lambda e: e.activation(out=lf[:], in_=lf[:], func=AF.Ln, bias=1.0, scale=1.0), reads=["lf"], writes=["lf"])
        P.op("dve", lambda e: e.tensor_scalar(out=lf[:], in0=lf[:], scalar1=-1.0, scalar2=None, op0=ALU.mult), reads=["lf"], writes=["lf"])
        P.op("pe", lambda e: e.matmul(pc[:, 0, :], lhsT=tri[:], rhs=lf[:], start=True, stop=True), reads=["tri", "lf"], writes=["pc0", "pcb"])
        state["f32mm"] = P.op("pe", lambda e: e.matmul(pc[:, 1, :], lhsT=onesf[:], rhs=lf[:], start=True, stop=True), reads=["onesf", "lf"], writes=["pc1", "pcb"])
        P.op("dve", lambda e: e.tensor_copy(out=sc[0][:], in_=pc[:, 1, :]), reads=["pc1", "pcb"], writes=["sc0"])
        cur = 0
        dd = 1
        while dd < 64:
            a, b = sc[cur], sc[1 - cur]
            an, bn = f"sc{cur}", f"sc{1 - cur}"
            P.op("dve", lambda e, a=a, b=b, dd=dd: e.tensor_copy(out=b[:, 0:dd], in_=a[:, 0:dd]), reads=[an], writes=[bn])
            P.op("dve", lambda e, a=a, b=b, dd=dd: e.tensor_tensor(out=b[:, dd:64], in0=a[:, dd:64], in1=a[:, 0:64 - dd], op=ALU.add),
                 reads=[an, bn], writes=[bn])
            cur = 1 - cur
            dd *= 2
        inc = sc[cur]; incn = f"sc{cur}"
        P.op("dve", lambda e, inc=inc: e.tensor_tensor(out=ex[:], in0=inc[:], in1=pc[:, 1, :], op=ALU.subtract), reads=[incn, "pc1", "pcb"], writes=["ex"])
        P.op("dve", lambda e: e.tensor_tensor(out=ccol[:], in0=pc[:, 0, :], in1=ex[:], op=ALU.add), reads=["pc0", "pcb", "ex"], writes=["ccol"])
        for qt in range(32):
            P.op("dve", lambda e, qt=qt: e.tensor_scalar(out=G[:, qt, :], in0=ccol[:], scalar1=-1.0, scalar2=ex[:, 2 * qt:2 * qt + 1],
                                                         op0=ALU.mult, op1=ALU.add), reads=["ccol", "ex"], writes=[("G", qt)])

    def run_maps(maps, V, vname, dv, epilogue, nq=32):
        nm = len(maps)
        for qt in range(nq):
            qtiles = []
            for (ki, qi) in maps:
                s = state["q"] % 4; state["q"] += 1
                P.dma("sp", lambda e, s=s, qi=qi, qt=qt: e.dma_start(out=qt_[s][:], in_=qkT[qi, :, qt * 256:(qt + 1) * 256]), writes=[f"qt{s}"])
                qtiles.append(s)
            nkb = 2 * qt + 2
            steps = [(kb, m) for kb in range(nkb) for m in range(nm)]
            sslot = {}
            def issue_S(idx):
                kb, m = steps[idx]
                s = state["s"] % 3; state["s"] += 1
                sslot[idx] = s
                ki = maps[m][0]
                lo = 128 if kb == nkb - 1 else 0
                qb = qtiles[m]
                P.op("pe", lambda e, s=s, ki=ki, kb=kb, qb=qb, lo=lo: e.matmul(ps_s[:, s, lo:256], lhsT=KT[:, ki, kb * 128:(kb + 1) * 128],
                                                                              rhs=qt_[qb][:, lo:256], start=True, stop=True),
                     reads=[("KT", ki), f"qt{qb}"], writes=[("ps_s", s)], after=[state["f32mm"]] if "f32mm" in state else [])
            LA = 2
            for idx in range(min(LA, len(steps))):
                issue_S(idx)
            for idx, (kb, m) in enumerate(steps):
                if idx + LA < len(steps):
                    issue_S(idx + LA)
                s = sslot[idx]
                p = state["p"] % 4; state["p"] += 1
                lo = 128 if kb == nkb - 1 else 0
                P.op("act", lambda e, s=s, p=p, kb=kb, qt=qt, lo=lo: e.activation(out=pt[p][:, lo:256], in_=ps_s[:, s, lo:256], func=AF.Exp,
                                                                                bias=G[:, qt, kb:kb + 1], scale=SCALE),
                     reads=[("ps_s", s), ("G", qt)], writes=[f"pt{p}"])
                if kb >= nkb - 2:
                    j = kb - (nkb - 2)
                    P.op("dve", lambda e, p=p, j=j: e.tensor_tensor(out=pt[p][:, j * 128:(j + 1) * 128], in0=pt[p][:, j * 128:(j + 1) * 128],
                                                                    in1=trib[:], op=ALU.mult), reads=[f"pt{p}", "trib"], writes=[f"pt{p}"])
                for j in range(2):
                    if j == 0 and kb == nkb - 1:
                        continue
                    last = (kb == nkb - 2) if j == 0 else (kb == nkb - 1)
                    r = m * 2 + j
                    P.op("pe", lambda e, p=p, j=j, kb=kb, r=r, last=last: e.matmul(po[:, r, 0:dv + 1], lhsT=pt[p][:, j * 128:(j + 1) * 128],
                                                                                   rhs=V[:, kb, :], start=(kb == 0), stop=last),
                         reads=[f"pt{p}", (vname, kb // 16), vname + "o"], writes=[("po", r)])
            epilogue(qt)

    def fox_epilogue(h):
        def ep(qt):
            o = state["o"] % 2; state["o"] += 1
            for j in range(2):
                r = state["r"] % 4; state["r"] += 1
                P.op("dve", lambda e, r=r, j=j: e.reciprocal(out=rec[r][:], in_=po[:, j, 128:129]), reads=[("po", j)], writes=[f"rec{r}"])
                P.op("dve", lambda e, r=r, j=j, o=o: e.tensor_scalar(out=ost[o][:, j, 0:128], in0=po[:, j, 0:128], scalar1=rec[r][:], scalar2=None,
                                                                     op0=ALU.mult), reads=[("po", j), f"rec{r}"], writes=[f"ost{o}"])
            P.dma("pool", lambda e, o=o, qt=qt: e.dma_start(
                out=o_out[qt * 256:(qt + 1) * 256, h * 128:(h + 1) * 128].rearrange("(j p) c -> p j c", p=128), in_=ost[o][:, :, 0:128]),
                reads=[f"ost{o}"], writes=[("o_out", h, qt)], is_output=out_is_final)
        return ep

    def diff_epilogue(qt):
        o = state["o"] % 2; state["o"] += 1
        for j in range(2):
            r0 = state["r"] % 4; state["r"] += 1
            r1 = state["r"] % 4; state["r"] += 1
            P.op("dve", lambda e, r0=r0, j=j: e.reciprocal(out=rec[r0][:], in_=po[:, j, 256:257]), reads=[("po", j)], writes=[f"rec{r0}"])
            P.op("dve", lambda e, r1=r1, j=j: e.reciprocal(out=rec[r1][:], in_=po[:, 2 + j, 256:257]), reads=[("po", 2 + j)], writes=[f"rec{r1}"])
            P.op("dve", lambda e, r1=r1: e.tensor_tensor(out=rec[r1][:], in0=rec[r1][:], in1=neglam[:], op=ALU.mult), reads=[f"rec{r1}", "neglam"], writes=[f"rec{r1}"])
            P.op("dve", lambda e, r0=r0, j=j: e.tensor_scalar(out=t0[j][:], in0=po[:, j, 0:256], scalar1=rec[r0][:], scalar2=None, op0=ALU.mult),
                 reads=[("po", j), f"rec{r0}"], writes=[f"t0{j}"])
            P.op("dve", lambda e, r1=r1, j=j: e.scalar_tensor_tensor(out=t1[j][:], in0=po[:, 2 + j, 0:256], scalar=rec[r1][:], in1=t0[j][:],
                                                                     op0=ALU.mult, op1=ALU.add), reads=[("po", 2 + j), f"rec{r1}", f"t0{j}"], writes=[f"t1{j}"])
            P.op("act", lambda e, j=j: e.activation(out=junk2[:], in_=t1[j][:], func=AF.Square, accum_out=ssd[j][:]), reads=[f"t1{j}"], writes=["junk2", f"ssd{j}"])
            P.op("act", lambda e, j=j: e.activation(out=ssd[j][:], in_=ssd[j][:], func=AF.Sqrt, bias=1e-5, scale=1.0 / 256), reads=[f"ssd{j}"], writes=[f"ssd{j}"])
            P.op("dve", lambda e, j=j: e.reciprocal(out=ssd[j][:], in_=ssd[j][:]), reads=[f"ssd{j}"], writes=[f"ssd{j}"])
            P.op("dve", lambda e, j=j, o=o: e.scalar_tensor_tensor(out=ost[o][:, j, :], in0=t1[j][:], scalar=ssd[j][:], in1=gs[:], op0=ALU.mult, op1=ALU.mult),
                 reads=[f"t1{j}", f"ssd{j}", "gs"], writes=[f"ost{o}"])
        P.dma("pool", lambda e, o=o, qt=qt: e.dma_start(
            out=o_out[qt * 256:(qt + 1) * 256, 256:512].rearrange("(j p) c -> p j c", p=128), in_=ost[o][:]),
            reads=[f"ost{o}"], writes=[("o_out", 2, qt)], is_output=out_is_final)

    Gall = [("G", qt) for qt in range(32)]
    for h in do_fox:
        fox_table(h)
        run_maps([(h, h)], Vf[h], f"Vf{h}", 128, fox_epilogue(h), nq=nq)
    if do_diff:
        P.dma("sp", lambda e: e.dma_start(out=G[:].rearrange("p a b -> p (a b)"), in_=alibi_d[:, :]), writes=Gall)
    if dbg is not None:
        P.dma("sp", lambda e: e.dma_start(out=dbg[:, 0:2048], in_=G[:].rearrange("p a b -> p (a b)")), reads=Gall, writes=["dbg0"], is_output=True)
        P.dma("sp", lambda e: e.dma_start(out=dbg[:, 2048:2112], in_=ccol[:]), reads=["ccol"], writes=["dbg1"], is_output=True)
        P.dma("sp", lambda e: e.dma_start(out=dbg[:, 2112:2176], in_=ex[:]), reads=["ex"], writes=["dbg2"], is_output=True)
        P.dma("sp", lambda e: e.dma_start(out=dbg[:, 2176:2240], in_=lf[:]), reads=["lf"], writes=["dbg3"], is_output=True)
    if do_diff:
        run_maps([(2, 2), (3, 3)], Vd, "Vd", 256, diff_epilogue, nq=nq)
    P.end_phase(final=out_is_final)


def build_l1(debug=False, only_proj=False):
    nc = bass.Bass("TRN2", target_bir_lowering=False)
    x = nc.dram_tensor("x", [S, D], F32, kind="ExternalInput").ap()
    wc = nc.dram_tensor("wc", [D, NWC], F32, kind="ExternalInput").ap()
    gcol = nc.dram_tensor("gcol", [128, 16], F32, kind="ExternalInput").ap()
    ident = nc.dram_tensor("ident", [128, 128], BF16, kind="ExternalInput").ap()
    fbias = nc.dram_tensor("fbias", [1, 2], F32, kind="ExternalInput").ap()
    alibi = nc.dram_tensor("alibi", [128, 2048], F32, kind="ExternalInput").ap()
    tri = nc.dram_tensor("tri", [128, 128], F32, kind="ExternalInput").ap()
    ones = nc.dram_tensor("ones", [128, 128], F32, kind="ExternalInput").ap()
    lamv = nc.dram_tensor("lamv", [1, 4, 128], F32, kind="ExternalInput").ap()
    sublng = nc.dram_tensor("sublng", [1, 256], F32, kind="ExternalInput").ap()
    kind = "ExternalOutput" if debug else "Internal"
    qkT = nc.dram_tensor("qkT", [8, 128, S], BF16, kind=kind).ap()
    vdr = nc.dram_tensor("vdr", [S, 512], BF16, kind=kind).ap()
    ffd = nc.dram_tensor("ffd", [128, 128], F32, kind=kind).ap()
    o = nc.dram_tensor("o", [S, 512], BF16, kind="ExternalOutput").ap()
    P = Prog(nc)
    phase_proj(P, nc, x, wc, gcol, ident, qkT, vdr, ffd)
    if only_proj:
        P.es.close()
        return nc
    phase_attn(P, nc, qkT, vdr, ffd, fbias, alibi, tri, ones, lamv, sublng, o, True)
    return nc


def l1_inputs(inp):
    x = inp["x"]
    w_in = inp["w_in"][0]
    offs = np.cumsum([0, 1024, 1024, 1024, 8, 1024, 1024, 1024])
    g = inp["attn_norm_g"][0]
    gcol = np.ascontiguousarray(g.reshape(16, 128).T)
    ident = np.eye(128, dtype=np.float32).astype(NPBF)
    tri = np.triu(np.ones((128, 128), np.float32))
    ones = np.ones((128, 128), np.float32)
    lamv = np.stack([inp["lambda_q1"][0], inp["lambda_k1"][0], inp["lambda_q2"][0], inp["lambda_k2"][0]])[None]
    slopes = 2.0 ** (-8.0 * np.arange(1, 5) / 4)
    p = np.arange(128, dtype=np.float32)[:, None, None]
    qt = np.arange(32, dtype=np.float32)[None, :, None]
    kb = np.arange(64, dtype=np.float32)[None, None, :]
    rel = kb * 128 + p - qt * 256
    maps = []
    for c in range(NCORES):
        b, j = c // 4, c % 4
        cols = np.concatenate([
            np.arange(offs[0] + 2 * j * 128, offs[0] + (2 * j + 2) * 128), np.arange(offs[4] + j * 256, offs[4] + (j + 1) * 256),
            np.arange(offs[1] + 2 * j * 128, offs[1] + (2 * j + 2) * 128), np.arange(offs[5] + j * 256, offs[5] + (j + 1) * 256),
            np.arange(offs[2] + 2 * j * 128, offs[2] + (2 * j + 2) * 128), np.arange(offs[6] + j * 256, offs[6] + (j + 1) * 256),
            np.arange(offs[3] + 2 * j, offs[3] + 2 * j + 2)])
        maps.append({
            "x": np.ascontiguousarray(x[b]),
            "wc": np.ascontiguousarray(w_in[:, cols]),
            "gcol": gcol, "ident": ident,
            "fbias": np.ascontiguousarray(inp["forget_bias"][0][2 * j:2 * j + 2][None]),
            "alibi": np.ascontiguousarray((np.float32(slopes[j]) * rel).astype(np.float32).reshape(128, 2048)),
            "tri": tri, "ones": ones, "lamv": np.ascontiguousarray(lamv.astype(np.float32)),
            "sublng": np.ascontiguousarray(inp["diff_subln_g"]),
        })
    return maps


def build_attn_only(nq=32, do_fox=(0, 1), do_diff=True):
    nc = bass.Bass("TRN2", target_bir_lowering=False)
    fbias = nc.dram_tensor("fbias", [1, 2], F32, kind="ExternalInput").ap()
    alibi = nc.dram_tensor("alibi", [128, 2048], F32, kind="ExternalInput").ap()
    tri = nc.dram_tensor("tri", [128, 128], F32, kind="ExternalInput").ap()
    ones = nc.dram_tensor("ones", [128, 128], F32, kind="ExternalInput").ap()
    lamv = nc.dram_tensor("lamv", [1, 4, 128], F32, kind="ExternalInput").ap()
    sublng = nc.dram_tensor("sublng", [1, 256], F32, kind="ExternalInput").ap()
    qkT = nc.dram_tensor("qkT", [8, 128, S], BF16, kind="ExternalInput").ap()
    vdr = nc.dram_tensor("vdr", [S, 512], BF16, kind="ExternalInput").ap()
    ffd = nc.dram_tensor("ffd", [128, 128], F32, kind="ExternalInput").ap()
    o = nc.dram_tensor("o", [S, 512], BF16, kind="ExternalOutput").ap()
    dbg = nc.dram_tensor("dbg", [128, 2240], F32, kind="ExternalOutput").ap()
    P = Prog(nc)
    phase_attn(P, nc, qkT, vdr, ffd, fbias, alibi, tri, ones, lamv, sublng, o, True, nq=nq, do_fox=do_fox, do_diff=do_diff, dbg=dbg)
    return nc


NT = 2048


def build_l2a(with_router=False):
    nc = bass.Bass("TRN2", target_bir_lowering=False)
    oT = nc.dram_tensor("oT", [16, 128, NT], BF16, kind="ExternalInput").ap()
    xs = nc.dram_tensor("xs", [NT, D], F32, kind="ExternalInput").ap()
    wo = nc.dram_tensor("wo", [D, D], F32, kind="ExternalInput").ap()
    gf = nc.dram_tensor("gf", [1, D], F32, kind="ExternalInput").ap()
    h_o = nc.dram_tensor("h", [NT, D], F32, kind="ExternalOutput").ap()
    hnf_o = nc.dram_tensor("hnf", [NT, D], F32, kind="ExternalOutput").ap()
    hnb_o = nc.dram_tensor("hnb", [NT, D], BF16, kind="ExternalOutput").ap()
    if with_router:
        wr = nc.dram_tensor("wr", [D, 72], F32, kind="ExternalInput").ap()
        br = nc.dram_tensor("br", [1, 72], F32, kind="ExternalInput").ap()
        io8 = nc.dram_tensor("io8", [1, 8], F32, kind="ExternalInput").ap()
        idf = nc.dram_tensor("identf", [128, 128], F32, kind="ExternalInput").ap()
        ri_o = nc.dram_tensor("ri", [NT, 8], F32, kind="ExternalOutput").ap()
    P = Prog(nc)
    P.begin_phase()
    wob = P.sb("wob", [128, 16, D], BF16)
    wst = [P.sb(f"wst{i}", [128, D], F32) for i in range(2)]
    g = P.sb("g", [128, D], F32)
    oTt = [P.sb(f"oTt{i}", [128, 16, 128], BF16) for i in range(2)]
    xt = [P.sb(f"xt{i}", [128, D], F32) for i in range(2)]
    ht = [P.sb(f"ht{i}", [128, D], F32) for i in range(2)]
    hn = [P.sb(f"hn{i}", [128, D], F32) for i in range(2)]
    hb = [P.sb(f"hb{i}", [128, D], BF16) for i in range(2)]
    junk = P.sb("junk", [128, D], BF16)
    ss = [P.sb(f"ss{i}", [128, 1], F32) for i in range(2)]
    nring = 5 if with_router else 8
    ph = P.ps("ph", [128, nring, 512], F32)
    ring = [0]
    if with_router:
        wrs = P.sb("wrs", [128, 16, 72], F32); brs = P.sb("brs", [128, 72], F32); iota = P.sb("iota", [128, 8], F32)
        identf = P.sb("identf", [128, 128], F32)
        hT32 = P.sb("hT32", [128, 16, 128], F32)
        ptf = P.ps("ptf", [128, 4, 128], F32)
        plog = P.ps("plog", [128, 2, 512], F32)
        P.dma("sp", lambda e: e.dma_start(out=wrs[:], in_=wr.rearrange("(c p) n -> p c n", p=128)), writes=["wrs"])
        P.dma("sp", lambda e: e.dma_start(out=brs[:], in_=br[0:1, :].broadcast_to([128, 72])), writes=["brs"])
        P.dma("sp", lambda e: e.dma_start(out=iota[:], in_=io8[0:1, :].broadcast_to([128, 8])), writes=["iota"])
        P.dma("sp", lambda e: e.dma_start(out=identf[:], in_=idf[:, :]), writes=["identf"])
    P.dma("sp", lambda e: e.dma_start(out=g[:], in_=gf[0:1, :].broadcast_to([128, D])), writes=["g"])
    for c in range(16):
        st = wst[c % 2]
        P.dma("sp", lambda e, st=st, c=c: e.dma_start(out=st[:], in_=wo[c * 128:(c + 1) * 128, :]), writes=[f"wst{c % 2}"])
        P.op("pool", lambda e, st=st, c=c: e.tensor_copy(out=wob[:, c, :], in_=st[:]), reads=[f"wst{c % 2}"], writes=[("wob", c)])
    wall = [("wob", c) for c in range(16)]
    for ti in range(NT // 128):
        b = ti % 2
        P.dma("sp", lambda e, b=b, ti=ti: e.dma_start(out=oTt[b][:], in_=oT[:, :, ti * 128:(ti + 1) * 128].rearrange("c p t -> p c t")), writes=[f"oTt{b}"])
        P.dma("sp", lambda e, b=b, ti=ti: e.dma_start(out=xt[b][:], in_=xs[ti * 128:(ti + 1) * 128, :]), writes=[f"xt{b}"])
        for cc in range(4):
            for c in range(16):
                if c == 0:
                    pbk = ring[0] % nring; ring[0] += 1
                P.op("pe", lambda e, b=b, cc=cc, c=c, pbk=pbk: e.matmul(ph[:, pbk, :], lhsT=oTt[b][:, c, :], rhs=wob[:, c, cc * 512:(cc + 1) * 512],
                                                                        start=(c == 0), stop=(c == 15)), reads=wall + [f"oTt{b}"], writes=[("ph", pbk)])
            P.op("dve", lambda e, b=b, cc=cc, pbk=pbk: e.tensor_tensor(out=ht[b][:, cc * 512:(cc + 1) * 512], in0=ph[:, pbk, :], in1=xt[b][:, cc * 512:(cc + 1) * 512], op=ALU.add),
                 reads=[("ph", pbk), f"xt{b}"], writes=[(f"ht{b}", cc)])
        hall = [(f"ht{b}", cc) for cc in range(4)]
        P.dma("pool", lambda e, b=b, ti=ti: e.dma_start(out=h_o[ti * 128:(ti + 1) * 128, :], in_=ht[b][:]), reads=hall, writes=[("h_o", ti)], is_output=True)
        P.op("act", lambda e, b=b: e.activation(out=junk[:], in_=ht[b][:], func=AF.Square, accum_out=ss[b][:]), reads=hall, writes=["junk", f"ss{b}"])
        P.op("act", lambda e, b=b: e.activation(out=ss[b][:], in_=ss[b][:], func=AF.Sqrt, bias=1e-6, scale=1.0 / D), reads=[f"ss{b}"], writes=[f"ss{b}"])
        P.op("dve", lambda e, b=b: e.reciprocal(out=ss[b][:], in_=ss[b][:]), reads=[f"ss{b}"], writes=[f"ss{b}"])
        P.op("dve", lambda e, b=b: e.scalar_tensor_tensor(out=hn[b][:], in0=ht[b][:], scalar=ss[b][:], in1=g[:], op0=ALU.mult, op1=ALU.mult),
             reads=hall + [f"ss{b}", "g"], writes=[f"hn{b}"])
        P.op("act", lambda e, b=b: e.copy(out=hb[b][:], in_=hn[b][:]), reads=[f"hn{b}"], writes=[f"hb{b}"])
        P.dma("pool", lambda e, b=b, ti=ti: e.dma_start(out=hnf_o[ti * 128:(ti + 1) * 128, :], in_=hn[b][:]), reads=[f"hn{b}"], writes=[("hnf_o", ti)], is_output=True)
        P.dma("pool", lambda e, b=b, ti=ti: e.dma_start(out=hnb_o[ti * 128:(ti + 1) * 128, :], in_=hb[b][:]), reads=[f"hb{b}"], writes=[("hnb_o", ti)], is_output=True)
        if with_router:
            for q4 in range(4):
                for i4 in range(4):
                    c = q4 * 4 + i4
                    P.op("pe", lambda e, b=b, c=c, i4=i4: e.transpose(ptf[:, i4, :], hn[b][:, c * 128:(c + 1) * 128], identf[:]),
                         reads=[f"hn{b}", "identf"], writes=["ptf"])
                P.op("act", lambda e, q4=q4: e.copy(out=hT32[:, q4 * 4:(q4 + 1) * 4, :], in_=ptf[:]), reads=["ptf"], writes=[("hT32", q4)])
            for c in range(16):
                P.op("pe", lambda e, b=b, c=c: e.matmul(plog[:, b, 0:72], lhsT=hT32[:, c, :], rhs=wrs[:, c, :], start=(c == 0), stop=(c == 15)),
                     reads=[("hT32", q4) for q4 in range(4)] + ["wrs"], writes=[("plog", b)])
            emit_router(P, ti, b, plog, brs, iota, ri_o)
    P.end_phase(final=True)
    return nc


def emit_router(P, ti, b, plog, brs, iota, ri_o):
    if True:
        T = {}
        def sbt(nm, w):
            T[nm] = P.sb(f"{nm}_{ti}", [128, w], F32)
            return T[nm]
        lg = sbt("lg", 72); goh = sbt("goh", 8); gex = sbt("gex", 8); t8 = sbt("t8", 8); sel = sbt("sel", 8)
        oh1 = sbt("oh1", 8); sel2 = sbt("sel2", 8); oh2 = sbt("oh2", 8); s1 = sbt("s1", 8); ri = sbt("ri", 8)
        rn = lambda k: f"{k}_{ti}"
        def dv(fn, reads, writes):
            P.op("dve", fn, reads=[rn(r) if isinstance(r, str) and not r.startswith("@") else (r[1:] if isinstance(r, str) else r) for r in reads],
                 writes=[rn(w) for w in writes])
        dv(lambda e, b=b, lg=lg: e.tensor_tensor(out=lg[:], in0=plog[:, b, 0:72], in1=brs[:], op=ALU.add), [("plog", b), "@brs"], ["lg"])
        dv(lambda e, lg=lg, s1=s1: e.tensor_reduce(out=s1[:, 0:1], in_=lg[:, 0:8], axis=AX.X, op=ALU.max), ["lg"], ["gm"])
        dv(lambda e, lg=lg, s1=s1, goh=goh: e.tensor_scalar(out=goh[:], in0=lg[:, 0:8], scalar1=s1[:, 0:1], scalar2=None, op0=ALU.is_equal), ["lg", "gm"], ["goh"])
        dv(lambda e, s1=s1: e.tensor_scalar(out=s1[:, 1:2], in0=s1[:, 0:1], scalar1=-1.0, scalar2=None, op0=ALU.mult), ["gm"], ["ngm"])
        P.op("act", lambda e, lg=lg, s1=s1, gex=gex: e.activation(out=gex[:], in_=lg[:, 0:8], func=AF.Exp, bias=s1[:, 1:2], scale=1.0, accum_out=s1[:, 2:3]),
             reads=[rn("lg"), rn("ngm")], writes=[rn("gex"), rn("gsum")])
        dv(lambda e, s1=s1: e.reciprocal(out=s1[:, 3:4], in_=s1[:, 2:3]), ["gsum"], ["gprob"])
        dv(lambda e, goh=goh, t8=t8: e.tensor_tensor(out=t8[:], in0=goh[:], in1=iota[:], op=ALU.mult), ["goh", "@iota"], ["t8"])
        dv(lambda e, t8=t8, ri=ri: e.tensor_reduce(out=ri[:, 0:1], in_=t8[:], axis=AX.X, op=ALU.add), ["t8"], ["ri0"])
        for gi in range(8):
            if gi == 0:
                dv(lambda e, lg=lg, goh=goh, sel=sel: e.tensor_scalar(out=sel[:], in0=lg[:, 8:16], scalar1=goh[:, 0:1], scalar2=None, op0=ALU.mult), ["lg", "goh"], ["sel"])
            else:
                dv(lambda e, lg=lg, goh=goh, sel=sel, gi=gi: e.scalar_tensor_tensor(out=sel[:], in0=lg[:, 8 + gi * 8:16 + gi * 8], scalar=goh[:, gi:gi + 1], in1=sel[:],
                                                                                  op0=ALU.mult, op1=ALU.add), ["lg", "goh", "sel"], ["sel"])
        dv(lambda e, sel=sel, s1=s1: e.tensor_reduce(out=s1[:, 4:5], in_=sel[:], axis=AX.X, op=ALU.max), ["sel"], ["m1"])
        dv(lambda e, sel=sel, s1=s1, oh1=oh1: e.tensor_scalar(out=oh1[:], in0=sel[:], scalar1=s1[:, 4:5], scalar2=None, op0=ALU.is_equal), ["sel", "m1"], ["oh1"])
        dv(lambda e, sel=sel, oh1=oh1, sel2=sel2: e.scalar_tensor_tensor(out=sel2[:], in0=oh1[:], scalar=-1e30, in1=sel[:], op0=ALU.mult, op1=ALU.add), ["sel", "oh1"], ["sel2"])
        dv(lambda e, sel2=sel2, s1=s1: e.tensor_reduce(out=s1[:, 5:6], in_=sel2[:], axis=AX.X, op=ALU.max), ["sel2"], ["m2"])
        dv(lambda e, sel2=sel2, s1=s1, oh2=oh2: e.tensor_scalar(out=oh2[:], in0=sel2[:], scalar1=s1[:, 5:6], scalar2=None, op0=ALU.is_equal), ["sel2", "m2"], ["oh2"])
        dv(lambda e, oh1=oh1, t8=t8: e.tensor_tensor(out=t8[:], in0=oh1[:], in1=iota[:], op=ALU.mult), ["oh1", "@iota", "ri0"], ["t8"])
        dv(lambda e, t8=t8, ri=ri: e.tensor_reduce(out=ri[:, 1:2], in_=t8[:], axis=AX.X, op=ALU.add), ["t8"], ["ri1"])
        dv(lambda e, oh2=oh2, t8=t8: e.tensor_tensor(out=t8[:], in0=oh2[:], in1=iota[:], op=ALU.mult), ["oh2", "@iota", "ri1"], ["t8"])
        dv(lambda e, t8=t8, ri=ri: e.tensor_reduce(out=ri[:, 2:3], in_=t8[:], axis=AX.X, op=ALU.add), ["t8"], ["ri2"])
        dv(lambda e, s1=s1: e.tensor_tensor(out=s1[:, 6:7], in0=s1[:, 5:6], in1=s1[:, 4:5], op=ALU.subtract), ["m1", "m2"], ["dd"])
        P.op("act", lambda e, s1=s1: e.activation(out=s1[:, 6:7], in_=s1[:, 6:7], func=AF.Exp), reads=[rn("dd")], writes=[rn("dd")])
        dv(lambda e, s1=s1: e.tensor_scalar(out=s1[:, 7:8], in0=s1[:, 6:7], scalar1=1.0, scalar2=None, op0=ALU.add), ["dd"], ["w1"])
        dv(lambda e, s1=s1: e.reciprocal(out=s1[:, 7:8], in_=s1[:, 7:8]), ["w1"], ["w1"])
        dv(lambda e, s1=s1, ri=ri: e.tensor_tensor(out=ri[:, 3:4], in0=s1[:, 7:8], in1=s1[:, 3:4], op=ALU.mult), ["w1", "gprob"], ["ri3"])
        dv(lambda e, s1=s1, ri=ri: e.tensor_tensor(out=ri[:, 4:5], in0=ri[:, 3:4], in1=s1[:, 6:7], op=ALU.mult), ["ri3", "dd"], ["ri4"])
        dv(lambda e, ri=ri: e.memset(ri[:, 5:8], 0.0), [], ["ri5"])
        P.dma("sp", lambda e, ri=ri, ti=ti: e.dma_start(out=ri_o[ti * 128:(ti + 1) * 128, :], in_=ri[:]),
              reads=[rn(k) for k in ("ri0", "ri1", "ri2", "ri3", "ri4", "ri5")], writes=[("ri_o", ti)], is_output=True)


def build_l2b():
    nc = bass.Bass("TRN2", target_bir_lowering=False)
    hT = nc.dram_tensor("hT", [16, 128, NT], F32, kind="ExternalInput").ap()
    wr = nc.dram_tensor("wr", [D, 72], F32, kind="ExternalInput").ap()
    br = nc.dram_tensor("br", [1, 72], F32, kind="ExternalInput").ap()
    io8 = nc.dram_tensor("io8", [1, 8], F32, kind="ExternalInput").ap()
    ri_o = nc.dram_tensor("ri", [NT, 8], F32, kind="ExternalOutput").ap()
    P = Prog(nc)
    P.begin_phase()
    wrs = P.sb("wrs", [128, 16, 72], F32)
    brs = P.sb("brs", [128, 72], F32)
    iota = P.sb("iota", [128, 8], F32)
    hTt = [P.sb(f"hTt{i}", [128, 16, 128], F32) for i in range(2)]
    plog = P.ps("plog", [128, 2, 512], F32)
    P.dma("sp", lambda e: e.dma_start(out=wrs[:], in_=wr.rearrange("(c p) n -> p c n", p=128)), writes=["wrs"])
    P.dma("sp", lambda e: e.dma_start(out=brs[:], in_=br[0:1, :].broadcast_to([128, 72])), writes=["brs"])
    P.dma("sp", lambda e: e.dma_start(out=iota[:], in_=io8[0:1, :].broadcast_to([128, 8])), writes=["iota"])
    names = ["lg", "goh", "gex", "t8", "sel", "oh1", "sel2", "oh2"]
    for ti in range(NT // 128):
        b = ti % 2
        P.dma("sp", lambda e, b=b, ti=ti: e.dma_start(out=hTt[b][:], in_=hT[:, :, ti * 128:(ti + 1) * 128].rearrange("c p t -> p c t")), writes=[f"hTt{b}"])
        for c in range(16):
            P.op("pe", lambda e, b=b, c=c: e.matmul(plog[:, b, 0:72], lhsT=hTt[b][:, c, :], rhs=wrs[:, c, :], start=(c == 0), stop=(c == 15)),
                 reads=[f"hTt{b}", "wrs"], writes=[("plog", b)])
        emit_router(P, ti, b, plog, brs, iota, ri_o)
    P.end_phase(final=True)
    return nc


def build_l3(cap):
    ncb = cap // 128
    nc = bass.Bass("TRN2", target_bir_lowering=False)
    XT = nc.dram_tensor("XT", [8, 16, 128, cap], BF16, kind="ExternalInput").ap()
    gcol = nc.dram_tensor("gcol", [128, 8 * ncb], F32, kind="ExternalInput").ap()
    wg = nc.dram_tensor("wg", [8, D, 1024], F32, kind="ExternalInput").ap()
    wu = nc.dram_tensor("wu", [8, D, 1024], F32, kind="ExternalInput").ap()
    wd = nc.dram_tensor("wd", [8, 1024, D], F32, kind="ExternalInput").ap()
    Y = nc.dram_tensor("Y", [8, cap, D], F32, kind="ExternalOutput").ap()
    P = Prog(nc)
    P.begin_phase()
    wbuf = [P.sb(f"wbuf{i}", [128, 16384], BF16) for i in range(4)]
    nxb = 2 if cap <= 640 else 1
    xT = [P.sb(f"xT{i}", [128, 16, cap], BF16) for i in range(nxb)]
    gc = P.sb("gc", [128, 8 * ncb], F32)
    nch = (cap + 511) // 512
    cw = ((ncb + nch - 1) // nch) * 128
    chunks = [(r0, min(cw, cap - r0)) for r0 in range(0, cap, cw)]
    pbi = [0]
    sg = [P.sb(f"sg{i}", [128, cw], F32) for i in range(2)]
    hdn = P.sb("hdn", [128, 8, cap], BF16)
    ys = [P.sb(f"ys{i}", [128, D], F32) for i in range(2)]
    pg = P.ps("pg", [128, 2, 512], F32)
    pu = P.ps("pu", [128, 2, 512], F32)
    py = P.ps("py", [128, 2, 512], F32)
    P.dma("sp", lambda e: e.dma_start(out=gc[:], in_=gcol[:, :]), writes=["gc"])
    wi = 0
    yi = 0
    for ex in range(8):
        xb = ex % nxb
        P.dma("sp", lambda e, xb=xb, ex=ex: e.dma_start(out=xT[xb][:], in_=XT[ex].rearrange("c p r -> p c r")), writes=[f"xT{xb}"])
        bufs = []
        for (w, kk) in ((wg, 16), (wu, 16), (wd, 8)):
            bi = wi % 4; wi += 1
            for half in range(2):
                k2 = kk // 2
                def ld(e, bi=bi, w=w, ex=ex, kk=kk, half=half, k2=k2):
                    ncol = 16384 // kk
                    dst = wbuf[bi][:].rearrange("p (c f) -> p c f", c=kk)[:, half * k2:(half + 1) * k2, :]
                    src = w[ex].rearrange("(c p) f -> p c f", p=128)[:, half * k2:(half + 1) * k2, :]
                    return e.dma_start(out=dst, in_=src)
                P.dma("pool", ld, writes=[(f"wbuf{bi}", half)])
            bufs.append(bi)
        bg, bu, bd = bufs
        wgv = wbuf[bg][:].rearrange("p (c f) -> p c f", c=16)
        wuv = wbuf[bu][:].rearrange("p (c f) -> p c f", c=16)
        wdv = wbuf[bd][:].rearrange("p (c f) -> p c f", c=8)
        for (r0, rw) in chunks:
            for f in range(8):
                pb = pbi[0] % 2; pbi[0] += 1
                for c in range(16):
                    P.op("pe", lambda e, pb=pb, c=c, f=f, wgv=wgv, xb=xb, r0=r0, rw=rw: e.matmul(pg[:, pb, 0:rw], lhsT=wgv[:, c, f * 128:(f + 1) * 128],
                                                                                               rhs=xT[xb][:, c, r0:r0 + rw], start=(c == 0), stop=(c == 15)),
                         reads=[(f"wbuf{bg}", 0), (f"wbuf{bg}", 1), f"xT{xb}"], writes=[("pg", pb)])
                for c in range(16):
                    P.op("pe", lambda e, pb=pb, c=c, f=f, wuv=wuv, xb=xb, r0=r0, rw=rw: e.matmul(pu[:, pb, 0:rw], lhsT=wuv[:, c, f * 128:(f + 1) * 128],
                                                                                               rhs=xT[xb][:, c, r0:r0 + rw], start=(c == 0), stop=(c == 15)),
                         reads=[(f"wbuf{bu}", 0), (f"wbuf{bu}", 1), f"xT{xb}"], writes=[("pu", pb)])
                P.op("act", lambda e, pb=pb, rw=rw: e.activation(out=sg[pb][:, 0:rw], in_=pg[:, pb, 0:rw], func=AF.Silu), reads=[("pg", pb)], writes=[f"sg{pb}"])
                P.op("dve", lambda e, pb=pb, f=f, r0=r0, rw=rw: e.tensor_tensor(out=hdn[:, f, r0:r0 + rw], in0=sg[pb][:, 0:rw], in1=pu[:, pb, 0:rw], op=ALU.mult),
                     reads=[f"sg{pb}", ("pu", pb)], writes=[("hdn", f, r0)])
        hall = [("hdn", f, r0) for f in range(8) for (r0, rw) in chunks]
        for rb in range(ncb):
            yb = yi % 2; yi += 1
            for cc in range(4):
                pb = cc % 2
                for f in range(8):
                    P.op("pe", lambda e, pb=pb, f=f, rb=rb, cc=cc, wdv=wdv: e.matmul(py[:, pb, :], lhsT=hdn[:, f, rb * 128:(rb + 1) * 128],
                                                                                     rhs=wdv[:, f, cc * 512:(cc + 1) * 512], start=(f == 0), stop=(f == 7)),
                         reads=hall + [(f"wbuf{bd}", 0), (f"wbuf{bd}", 1)], writes=[("py", pb)])
                gi = ex * ncb + rb
                P.op("dve", lambda e, pb=pb, yb=yb, cc=cc, gi=gi: e.tensor_scalar(out=ys[yb][:, cc * 512:(cc + 1) * 512], in0=py[:, pb, :], scalar1=gc[:, gi:gi + 1],
                                                                                  scalar2=None, op0=ALU.mult), reads=[("py", pb), "gc"], writes=[(f"ys{yb}", cc)])
            P.dma("sp", lambda e, yb=yb, ex=ex, rb=rb: e.dma_start(out=Y[ex, rb * 128:(rb + 1) * 128, :], in_=ys[yb][:]),
                  reads=[(f"ys{yb}", cc) for cc in range(4)], writes=[("Y", ex, rb)], is_output=True)
    P.end_phase(final=True)
    return nc


def build_l4():
    nc = bass.Bass("TRN2", target_bir_lowering=False)
    h = nc.dram_tensor("h", [NT, D], F32, kind="ExternalInput").ap()
    y1 = nc.dram_tensor("y1", [NT, D], F32, kind="ExternalInput").ap()
    y2 = nc.dram_tensor("y2", [NT, D], F32, kind="ExternalInput").ap()
    gf = nc.dram_tensor("gf", [1, D], F32, kind="ExternalInput").ap()
    out = nc.dram_tensor("out", [NT, D], F32, kind="ExternalOutput").ap()
    P = Prog(nc)
    P.begin_phase()
    g = P.sb("g", [128, D], F32)
    a = [P.sb(f"a{i}", [128, D], F32) for i in range(2)]
    b1 = [P.sb(f"b1{i}", [128, D], F32) for i in range(2)]
    b2 = [P.sb(f"b2{i}", [128, D], F32) for i in range(2)]
    ot = [P.sb(f"ot{i}", [128, D], F32) for i in range(2)]
    junk = P.sb("junk", [128, D], BF16)
    ss = [P.sb(f"ss{i}", [128, 1], F32) for i in range(2)]
    P.dma("sp", lambda e: e.dma_start(out=g[:], in_=gf[0:1, :].broadcast_to([128, D])), writes=["g"])
    for ti in range(NT // 128):
        b = ti % 2
        sl = slice(ti * 128, (ti + 1) * 128)
        P.dma("sp", lambda e, b=b, sl=sl: e.dma_start(out=a[b][:], in_=h[sl, :]), writes=[f"a{b}"])
        P.dma("sp", lambda e, b=b, sl=sl: e.dma_start(out=b1[b][:], in_=y1[sl, :]), writes=[f"b1{b}"])
        P.dma("sp", lambda e, b=b, sl=sl: e.dma_start(out=b2[b][:], in_=y2[sl, :]), writes=[f"b2{b}"])
        P.op("pool", lambda e, b=b: e.tensor_tensor(out=b1[b][:], in0=b1[b][:], in1=b2[b][:], op=ALU.add), reads=[f"b1{b}", f"b2{b}"], writes=[f"b1{b}"])
        P.op("dve", lambda e, b=b: e.tensor_tensor(out=a[b][:], in0=a[b][:], in1=b1[b][:], op=ALU.add), reads=[f"a{b}", f"b1{b}"], writes=[f"a{b}"])
        P.op("act", lambda e, b=b: e.activation(out=junk[:], in_=a[b][:], func=AF.Square, accum_out=ss[b][:]), reads=[f"a{b}"], writes=["junk", f"ss{b}"])
        P.op("act", lambda e, b=b: e.activation(out=ss[b][:], in_=ss[b][:], func=AF.Sqrt, bias=1e-6, scale=1.0 / D), reads=[f"ss{b}"], writes=[f"ss{b}"])
        P.op("dve", lambda e, b=b: e.reciprocal(out=ss[b][:], in_=ss[b][:]), reads=[f"ss{b}"], writes=[f"ss{b}"])
        P.op("dve", lambda e, b=b: e.scalar_tensor_tensor(out=ot[b][:], in0=a[b][:], scalar=ss[b][:], in1=g[:], op0=ALU.mult, op1=ALU.mult),
             reads=[f"a{b}", f"ss{b}", "g"], writes=[f"ot{b}"])
        P.dma("pool", lambda e, b=b, sl=sl: e.dma_start(out=out[sl, :], in_=ot[b][:]), reads=[f"ot{b}"], writes=[("out", ti)], is_output=True)
    P.end_phase(final=True)
    return nc


def _run(nc, maps):
    res = run_bass_kernel_spmd(nc, maps, core_ids=list(range(NCORES)))
    return res.results


def kernel(**inp):
    inp = {k: np.asarray(v) for k, v in inp.items()}
    N = NB * S
    r1 = _run(build_l1(), l1_inputs(inp))
    O = np.empty((NB, S, D), dtype=NPBF)
    for c in range(NCORES):
        b, j = c // 4, c % 4
        o = np.asarray(r1[c]["o"])
        if o.dtype != NPBF:
            o = o.view(NPBF)
        O[b, :, 2 * j * 128:(2 * j + 2) * 128] = o[:, 0:256]
        O[b, :, 1024 + j * 256:1024 + (j + 1) * 256] = o[:, 256:512]
    Of = O.reshape(N, D)
    xf = inp["x"].reshape(N, D)
    wr = np.ascontiguousarray(np.concatenate([inp["router_group_w"][0], inp["router_expert_w"][0]], 1))
    br = np.ascontiguousarray(np.concatenate([inp["router_group_b"][0], inp["router_expert_b"][0]])[None])
    io8 = np.arange(8, dtype=np.float32)[None]
    identf = np.eye(128, dtype=np.float32)
    maps = []
    for c in range(NCORES):
        sl = slice(c * NT, (c + 1) * NT)
        maps.append({"oT": np.ascontiguousarray(Of[sl].T).reshape(16, 128, NT), "xs": np.ascontiguousarray(xf[sl]),
                     "wo": np.ascontiguousarray(inp["w_out"][0]), "gf": np.ascontiguousarray(inp["ffn_norm_g"]),
                     "wr": wr, "br": br, "io8": io8, "identf": identf})
    r2 = _run(build_l2a(with_router=True), maps)
    h = np.concatenate([np.asarray(r["h"]) for r in r2], 0)
    hnb = np.concatenate([np.asarray(r["hnb"]) for r in r2], 0)
    if hnb.dtype != NPBF:
        hnb = hnb.view(NPBF)
    ri = np.concatenate([np.asarray(r["ri"]) for r in r2], 0)
    gidx = np.rint(ri[:, 0]).astype(np.int64)
    eloc = np.rint(ri[:, 1:3]).astype(np.int64)
    gates = ri[:, 3:5]
    rows = [[[] for _ in range(8)] for _ in range(8)]
    slot = np.zeros((N, 2), np.int64)
    for k in range(2):
        for g in range(8):
            for e in range(8):
                idx = np.nonzero((gidx == g) & (eloc[:, k] == e))[0]
                base = len(rows[g][e])
                slot[idx, k] = base + np.arange(len(idx))
                rows[g][e].extend([(int(t), k) for t in idx])
    mx = max(len(rows[g][e]) for g in range(8) for e in range(8))
    cap = max(128, ((mx + 127) // 128) * 128)
    ncb = cap // 128
    maps = []
    for g in range(8):
        XT = np.zeros((8, cap, D), dtype=NPBF)
        gc = np.zeros((8, cap), np.float32)
        for e in range(8):
            if rows[g][e]:
                t = np.array([a for a, _ in rows[g][e]]); kk = np.array([b for _, b in rows[g][e]])
                XT[e, :len(t)] = hnb[t]
                gc[e, :len(t)] = gates[t, kk]
        XTt = np.ascontiguousarray(XT.transpose(0, 2, 1)).reshape(8, 16, 128, cap)
        gcol = np.ascontiguousarray(gc.reshape(8, ncb, 128).transpose(2, 0, 1).reshape(128, 8 * ncb))
        maps.append({"XT": XTt, "gcol": gcol,
                     "wg": np.ascontiguousarray(inp["w_gate"][0, g * 8:(g + 1) * 8]), "wu": np.ascontiguousarray(inp["w_up"][0, g * 8:(g + 1) * 8]),
                     "wd": np.ascontiguousarray(inp["w_down"][0, g * 8:(g + 1) * 8])})
    r4 = _run(build_l3(cap), maps)
    Yall = np.stack([np.asarray(r["Y"]) for r in r4], 0)
    y1 = Yall[gidx, eloc[:, 0], slot[:, 0]]
    y2 = Yall[gidx, eloc[:, 1], slot[:, 1]]
    maps = [{"h": np.ascontiguousarray(h[c * NT:(c + 1) * NT]), "y1": np.ascontiguousarray(y1[c * NT:(c + 1) * NT]),
             "y2": np.ascontiguousarray(y2[c * NT:(c + 1) * NT]), "gf": np.ascontiguousarray(inp["final_norm_g"][None])} for c in range(NCORES)]
    r5 = _run(build_l4(), maps)
    out = np.concatenate([np.asarray(r["out"]) for r in r5], 0).reshape(NB, S, D).astype(np.float32)
    return out
```

```python
from contextlib import ExitStack
import numpy as np
import ml_dtypes
import concourse.bass as bass
import concourse.mybir as mybir
from concourse.bass_utils import run_bass_kernel_spmd

F32 = mybir.dt.float32
BF16 = mybir.dt.bfloat16
I32 = mybir.dt.int32
AF = mybir.ActivationFunctionType
ALU = mybir.AluOpType
AX = mybir.AxisListType
NPBF = ml_dtypes.bfloat16

D = 2048
S = 8192
NB = 2
NCORES = 8
HD = 128
NWC = 1538
SCALE = HD ** -0.5
LAM_INIT = 0.2
ENGS = ("pe", "act", "dve", "pool", "sp")
NDMASEM = 8


class Prog:
    def __init__(self, nc):
        self.nc = nc
        self.es = ExitStack()
        self.ops = {e: [] for e in ENGS}
        self.cnt = {e: 0 for e in ENGS}
        self.sem = {e: self.es.enter_context(nc.semaphore("s_" + e)) for e in ENGS}
        self.dsem = {q: [self.es.enter_context(nc.semaphore(f"d_{q}{i}")) for i in range(NDMASEM)]
                     for q in ("sp", "pool")}
        self.dcnt = {"sp": 0, "pool": 0}
        self.known = {e: {} for e in ENGS}
        self.lastw = {}
        self.readers = {}
        self.final_waits = []
        self.phase_es = None

    def begin_phase(self):
        self.phase_es = ExitStack()

    def sb(self, name, shape, dt):
        return self.phase_es.enter_context(self.nc.sbuf_tensor("sb_" + name, list(shape), dt))

    def ps(self, name, shape, dt):
        return self.phase_es.enter_context(self.nc.psum_tensor("ps_" + name, list(shape), dt))

    def _deps(self, eng, reads, writes):
        deps = set()
        for r in reads:
            w = self.lastw.get(r)
            if w is not None:
                deps.add(w)
        for w_ in writes:
            w = self.lastw.get(w_)
            if w is not None:
                deps.add(w)
            for rd in self.readers.get(w_, ()):
                deps.add(rd)
        best = {}
        for d in deps:
            if d[0] == "pe" and eng == "pe":
                continue
            if self.known[eng].get(d[0], 0) >= d[1]:
                continue
            if best.get(d[0], 0) < d[1]:
                best[d[0]] = d[1]
        for s, v in best.items():
            self.known[eng][s] = v
        return list(best.items())

    def _mark(self, token, reads, writes):
        for r in reads:
            self.readers.setdefault(r, []).append(token)
        for w in writes:
            self.lastw[w] = token
            self.readers[w] = []

    def op(self, eng, fn, reads=(), writes=(), after=()):
        waits = self._deps(eng, reads, writes)
        for (src, v) in after:
            if self.known[eng].get(src, 0) < v:
                waits.append((src, v))
                self.known[eng][src] = v
        self.cnt[eng] += 1
        token = (eng, self.cnt[eng])
        self.ops[eng].append((waits, fn, ("eng", eng)))
        self._mark(token, reads, writes)
        return token

    def dma(self, q, fn, reads=(), writes=(), is_output=False, inc=16):
        waits = self._deps(q, reads, writes)
        i = self.dcnt[q]
        self.dcnt[q] += 1
        slot = i % NDMASEM
        val = 16 * (i // NDMASEM + 1)
        src = ("d", q, slot)
        if i >= NDMASEM and self.known[q].get(src, 0) < val - 16:
            waits.append((src, val - 16))
            self.known[q][src] = val - 16
        token = (src, val)
        self.ops[q].append((waits, fn, ("dma", q, slot, inc)))
        self._mark(token, reads, writes)
        if is_output:
            self.final_waits.append(token)
        return token

    def _semof(self, src):
        if isinstance(src, tuple):
            return self.csem[src[1]] if src[0] == "c" else self.dsem[src[1]][src[2]]
        return self.sem[src]

    def collective(self, fn, writes=(), inc=16):
        if not hasattr(self, "csem"):
            self.csem = []
        sem = self.es.enter_context(self.nc.semaphore(f"cc{len(self.csem)}"))
        self.csem.append(sem)
        k = len(self.csem) - 1
        fn(self.nc.gpsimd).then_inc(sem, inc)
        self.nc.gpsimd.wait_ge(sem, inc)
        token = (("c", k), inc)
        self._mark(token, (), writes)
        return token

    def end_phase(self, final=False):
        allw = [(e, self.cnt[e]) for e in ENGS if self.cnt[e] > 0]
        for q in ("sp", "pool"):
            n = self.dcnt[q]
            for slot in range(min(n, NDMASEM)):
                last_i = ((n - 1 - slot) // NDMASEM) * NDMASEM + slot
                allw.append((("d", q, slot), 16 * (last_i // NDMASEM + 1)))
        for e in ENGS:
            w = [(s, v) for (s, v) in allw if self.known[e].get(s, 0) < v]
            for s, v in w:
                self.known[e][s] = v
            self.ops[e].append((w, None, None))
        nc = self.nc
        with nc.Block() as block:
            def run(eng_name):
                def body(e):
                    for waits, fn, inc in self.ops[eng_name]:
                        for (src, val) in waits:
                            e.wait_ge(self._semof(src), val)
                        if fn is None:
                            continue
                        ins = fn(e)
                        if inc[0] == "eng":
                            ins.then_inc(self.sem[inc[1]], 1)
                        else:
                            ins.then_inc(self.dsem[inc[1]][inc[2]], inc[3])
                return body
            block.tensor(run("pe"))
            block.scalar(run("act"))
            block.vector(run("dve"))
            block.gpsimd(run("pool"))
            block.sync(run("sp"))
        self.ops = {e: [] for e in ENGS}
        self.phase_es.close()
        self.phase_es = None
        if final:
            self.es.close()


def phase_proj(P, nc, x, wc, gcol, ident_d, qkT, vdr, ffd):
    P.begin_phase()
    wb = P.sb("wb", [128, 16, NWC], BF16)
    grep_ = P.sb("grep", [128, D], F32)
    ident = P.sb("ident", [128, 128], BF16)
    xt = [P.sb(f"xt{i}", [128, D], F32) for i in range(3)]
    junk = P.sb("junk", [128, D], BF16)
    ab = [P.sb(f"ab{i}", [128, D], BF16) for i in range(2)]
    ss = [P.sb(f"ss{i}", [128, 1], F32) for i in range(2)]
    rstd = [P.sb(f"rstd{i}", [128, 1], F32) for i in range(2)]
    aT = [P.sb(f"aT{i}", [128, 16, 512], BF16) for i in range(2)]
    qkst = [P.sb(f"qkst{i}", [128, 8, 512], BF16) for i in range(2)]
    vst = [P.sb(f"vst{i}", [128, 512], BF16) for i in range(2)]
    ffs = P.sb("ffs", [128, 128], F32)
    ptr = P.ps("ptr", [128, D], BF16)
    pqk = P.ps("pqk", [128, 2, 512], F32)
    pv = P.ps("pv", [128, 2, 512], F32)
    pf = P.ps("pf", [128, 2, 512], F32)

    P.dma("sp", lambda e: e.dma_start(out=grep_[:], in_=gcol[0:1, :].broadcast_to([128, D])), writes=["grep"])
    P.dma("sp", lambda e: e.dma_start(out=ident[:], in_=ident_d[:, :]), writes=["ident"])
    for c in range(16):
        P.dma("pool", lambda e, c=c: e.dma_start(out=wb[:, c, :], in_=wc[c * 128:(c + 1) * 128, :]), writes=[("wb", c)])

    tile_i = 0
    for gi in range(16):
        aTg = aT[gi % 2]
        aTn = f"aT{gi % 2}"
        for tt in range(4):
            ti = gi * 4 + tt
            xs = xt[ti % 3]; xn = f"xt{ti % 3}"
            abt = ab[ti % 2]; abn = f"ab{ti % 2}"
            sst = ss[ti % 2]; ssn = f"ss{ti % 2}"
            rs = rstd[ti % 2]; rsn = f"rstd{ti % 2}"
            P.dma("sp", lambda e, xs=xs, ti=ti: e.dma_start(out=xs[:], in_=x[ti * 128:(ti + 1) * 128, :]), writes=[xn])
            P.op("act", lambda e, xs=xs, sst=sst: e.activation(out=junk[:], in_=xs[:], func=AF.Square, accum_out=sst[:]),
                 reads=[xn], writes=["junk", ssn])
            P.op("act", lambda e, sst=sst: e.activation(out=sst[:], in_=sst[:], func=AF.Sqrt, bias=1e-6, scale=1.0 / D),
                 reads=[ssn], writes=[ssn])
            P.op("dve", lambda e, sst=sst, rs=rs: e.reciprocal(out=rs[:], in_=sst[:]), reads=[ssn], writes=[rsn])
            P.op("dve", lambda e, xs=xs, rs=rs, abt=abt: e.scalar_tensor_tensor(out=abt[:], in0=xs[:], scalar=rs[:], in1=grep_[:], op0=ALU.mult, op1=ALU.mult),
                 reads=[xn, rsn, "grep"], writes=[abn])
            for c in range(16):
                P.op("pe", lambda e, c=c, abt=abt: e.transpose(ptr[:, c * 128:(c + 1) * 128], abt[:, c * 128:(c + 1) * 128], ident[:]),
                     reads=[abn, "ident"], writes=[("ptr", c // 8)])
            for hh in range(2):
                eng = "act" if hh == 0 else "dve"
                def cp(e, hh=hh, aTg=aTg, tt=tt, eng=eng):
                    o = aTg[:, hh * 8:(hh + 1) * 8, tt * 128:(tt + 1) * 128]
                    i = ptr[:, hh * 1024:(hh + 1) * 1024].rearrange("p (c t) -> p c t", c=8)
                    if eng == "act":
                        return e.copy(out=o, in_=i)
                    return e.tensor_copy(out=o, in_=i)
                P.op(eng, cp, reads=[("ptr", hh)], writes=[(aTn, tt)])
        aT_all = [(aTn, tt) for tt in range(4)]
        qs = qkst[gi % 2]; qsn = f"qkst{gi % 2}"
        for cb in range(8):
            bank = cb % 2
            for c in range(16):
                P.op("pe", lambda e, cb=cb, c=c, bank=bank, aTg=aTg: e.matmul(pqk[:, bank, :], lhsT=wb[:, c, cb * 128:(cb + 1) * 128],
                                                                              rhs=aTg[:, c, :], start=(c == 0), stop=(c == 15)),
                     reads=[("wb", c)] + aT_all, writes=[("pqk", bank)])
            eng = "act" if cb % 2 == 0 else "dve"
            def ev(e, cb=cb, bank=bank, qs=qs, eng=eng):
                if eng == "act":
                    return e.copy(out=qs[:, cb, :], in_=pqk[:, bank, :])
                return e.tensor_copy(out=qs[:, cb, :], in_=pqk[:, bank, :])
            P.op(eng, ev, reads=[("pqk", bank)], writes=[(qsn, cb)])
        P.dma("pool", lambda e, qs=qs, gi=gi: e.dma_start(out=qkT[:, :, gi * 512:(gi + 1) * 512].rearrange("c p t -> p c t"), in_=qs[:]),
              reads=[(qsn, cb) for cb in range(8)], writes=[("qkT", gi)])
        for tt in range(4):
            ti = gi * 4 + tt
            bank = ti % 2
            vs = vst[ti % 2]; vsn = f"vst{ti % 2}"
            for c in range(16):
                P.op("pe", lambda e, c=c, bank=bank, tt=tt, aTg=aTg: e.matmul(pv[:, bank, :], lhsT=aTg[:, c, tt * 128:(tt + 1) * 128],
                                                                              rhs=wb[:, c, 1024:1536], start=(c == 0), stop=(c == 15)),
                     reads=[("wb", c)] + aT_all, writes=[("pv", bank)])
            for c in range(16):
                P.op("pe", lambda e, c=c, bank=bank, tt=tt, aTg=aTg: e.matmul(pf[:, bank, 0:2], lhsT=aTg[:, c, tt * 128:(tt + 1) * 128],
                                                                              rhs=wb[:, c, 1536:1538], start=(c == 0), stop=(c == 15)),
                     reads=[("wb", c)] + aT_all, writes=[("pf", bank)])
            eng = "act" if tt % 2 == 0 else "dve"
            def evv(e, bank=bank, vs=vs, eng=eng):
                if eng == "act":
                    return e.copy(out=vs[:], in_=pv[:, bank, :])
                return e.tensor_copy(out=vs[:], in_=pv[:, bank, :])
            P.op(eng, evv, reads=[("pv", bank)], writes=[vsn])
            P.op("dve", lambda e, bank=bank, ti=ti: e.tensor_copy(out=ffs[:, ti * 2:ti * 2 + 2], in_=pf[:, bank, 0:2]),
                 reads=[("pf", bank)], writes=[("ffs", ti)])
            P.dma("pool", lambda e, vs=vs, ti=ti: e.dma_start(out=vdr[ti * 128:(ti + 1) * 128, :], in_=vs[:]),
                  reads=[vsn], writes=[("vdr", ti)])
    P.dma("pool", lambda e: e.dma_start(out=ffd[:, :], in_=ffs[:]), reads=[("ffs", ti) for ti in range(64)], writes=["ffd"])
    P.end_phase()


def phase_attn(P, nc, qkT, vdr, ffd, fbias, alibi_d, tri_d, ones_d, lamv, sublng, o_out, out_is_final, nq=32, do_fox=(0, 1), do_diff=True, dbg=None):
    P.begin_phase()
    KT = P.sb("KT", [128, 4, S], BF16)
    Vf = [P.sb(f"Vf{h}", [128, 64, 129], BF16) for h in range(2)]
    Vd = P.sb("Vd", [128, 64, 257], BF16)
    G = P.sb("G", [128, 32, 64], F32)
    tri = P.sb("tri", [128, 128], F32)
    trib = P.sb("trib", [128, 128], BF16)
    onesf = P.sb("onesf", [128, 128], F32)
    ff = P.sb("ff", [128, 128], F32)
    fb = P.sb("fb", [128, 2], F32)
    lf = P.sb("lf", [128, 64], F32)
    ccol = P.sb("ccol", [128, 64], F32)
    sc = [P.sb(f"sc{i}", [128, 64], F32) for i in range(2)]
    ex = P.sb("ex", [128, 64], F32)
    lv = P.sb("lv", [128, 4, 128], F32)
    lsc = P.sb("lsc", [128, 128], F32)
    l1 = P.sb("l1", [128, 1], F32)
    l2 = P.sb("l2", [128, 1], F32)
    neglam = P.sb("neglam", [128, 1], F32)
    gs = P.sb("gs", [128, 256], F32)
    qt_ = [P.sb(f"qt{i}", [128, 256], BF16) for i in range(4)]
    pt = [P.sb(f"pt{i}", [128, 256], BF16) for i in range(4)]
    ost = [P.sb(f"ost{i}", [128, 2, 256], BF16) for i in range(2)]
    rec = [P.sb(f"rec{i}", [128, 1], F32) for i in range(4)]
    t0 = [P.sb(f"t0{i}", [128, 256], F32) for i in range(2)]
    t1 = [P.sb(f"t1{i}", [128, 256], F32) for i in range(2)]
    junk2 = P.sb("junk2", [128, 256], F32)
    ssd = [P.sb(f"ssd{i}", [128, 1], F32) for i in range(2)]
    ps_s = P.ps("ps_s", [128, 3, 512], F32)
    po = P.ps("po", [128, 4, 512], F32)
    pc = P.ps("pc", [128, 2, 64], F32)

    for i in range(4):
        P.dma("sp", lambda e, i=i: e.dma_start(out=KT[:, i, :], in_=qkT[4 + i, :, :]), writes=[("KT", i)])
    vview = vdr.rearrange("(k p) c -> p k c", p=128)
    for h in range(2):
        for kq in range(4):
            P.dma("sp", lambda e, h=h, kq=kq: e.dma_start(out=Vf[h][:, kq * 16:(kq + 1) * 16, 0:128],
                                                         in_=vview[:, kq * 16:(kq + 1) * 16, h * 128:(h + 1) * 128]), writes=[(f"Vf{h}", kq)])
        P.op("pool", lambda e, h=h: e.memset(Vf[h][:, :, 128:129], 1.0), reads=[(f"Vf{h}", kq) for kq in range(4)], writes=[f"Vf{h}o"])
    for kq in range(4):
        P.dma("sp", lambda e, kq=kq: e.dma_start(out=Vd[:, kq * 16:(kq + 1) * 16, 0:256], in_=vview[:, kq * 16:(kq + 1) * 16, 256:512]), writes=[("Vd", kq)])
    P.op("pool", lambda e: e.memset(Vd[:, :, 256:257], 1.0), reads=[("Vd", kq) for kq in range(4)], writes=["Vdo"])
    P.dma("sp", lambda e: e.dma_start(out=tri[:], in_=tri_d[:, :]), writes=["tri"])
    P.dma("sp", lambda e: e.dma_start(out=onesf[:], in_=ones_d[:, :]), writes=["onesf"])
    P.dma("sp", lambda e: e.dma_start(out=ff[:], in_=ffd[:, :]), writes=["ff"])
    P.dma("sp", lambda e: e.dma_start(out=fb[:], in_=fbias[0:1, :].broadcast_to([128, 2])), writes=["fb"])
    P.dma("sp", lambda e: e.dma_start(out=lv[:], in_=lamv[0:1, :, :].broadcast_to([128, 4, 128])), writes=["lv"])
    P.dma("sp", lambda e: e.dma_start(out=gs[:], in_=sublng[0:1, :].broadcast_to([128, 256])), writes=["gs"])
    P.op("dve", lambda e: e.tensor_copy(out=trib[:], in_=tri[:]), reads=["tri"], writes=["trib"])
    P.op("dve", lambda e: e.tensor_scalar(out=fb[:], in0=fb[:], scalar1=-1.0, scalar2=None, op0=ALU.mult), reads=["fb"], writes=["fb"])
    P.op("dve", lambda e: e.tensor_tensor(out=lsc[:], in0=lv[:, 0, :], in1=lv[:, 1, :], op=ALU.mult), reads=["lv"], writes=["lsc"])
    P.op("dve", lambda e: e.tensor_reduce(out=l1[:], in_=lsc[:], axis=AX.X, op=ALU.add), reads=["lsc"], writes=["l1"])
    P.op("dve", lambda e: e.tensor_tensor(out=lsc[:], in0=lv[:, 2, :], in1=lv[:, 3, :], op=ALU.mult), reads=["lv", "l1"], writes=["lsc"])
    P.op("dve", lambda e: e.tensor_reduce(out=l2[:], in_=lsc[:], axis=AX.X, op=ALU.add), reads=["lsc"], writes=["l2"])
    P.op("act", lambda e: e.activation(out=l1[:], in_=l1[:], func=AF.Exp), reads=["l1"], writes=["l1"])
    P.op("act", lambda e: e.activation(out=l2[:], in_=l2[:], func=AF.Exp), reads=["l2"], writes=["l2"])
    P.op("dve", lambda e: e.tensor_tensor(out=neglam[:], in0=l2[:], in1=l1[:], op=ALU.subtract), reads=["l1", "l2"], writes=["neglam"])
    P.op("dve", lambda e: e.tensor_scalar(out=neglam[:], in0=neglam[:], scalar1=-LAM_INIT, scalar2=None, op0=ALU.add), reads=["neglam"], writes=["neglam"])
    P.op("dve", lambda e: e.tensor_scalar(out=gs[:], in0=gs[:], scalar1=1.0 - LAM_INIT, scalar2=None, op0=ALU.mult), reads=["gs"], writes=["gs"])

    state = {"q": 0, "p": 0, "s": 0, "o": 0, "r": 0}

    def fox_table(h):
        P.op("act", lambda e: e.activation(out=lf[:], in_=ff[:].rearrange("p (k h) -> p k h", h=2)[:, :, h], func=AF.Exp,
                                           bias=fb[:, h:h + 1], scale=-1.0), reads=["ff", "fb"], writes=["lf"])
        P.op("act", lambda e: e.activation(out=lf[:], in_=lf[:], func=AF.Ln, bias=1.0, scale=1.0), reads=["lf"], writes=["lf"])
        P.op("dve", lambda e: e.tensor_scalar(out=lf[:], in0=lf[:], scalar1=-1.0, scalar2=None, op0=ALU.mult), reads=["lf"], writes=["lf"])
        P.op("pe", lambda e: e.matmul(pc[:, 0, :], lhsT=tri[:], rhs=lf[:], start=True, stop=True), reads=["tri", "lf"], writes=["pc0", "pcb"])
        state["f32mm"] = P.op("pe", lambda e: e.matmul(pc[:, 1, :], lhsT=onesf[:], rhs=lf[:], start=True, stop=True), reads=["onesf", "lf"], writes=["pc1", "pcb"])
        P.op("dve", lambda e: e.tensor_copy(out=sc[0][:], in_=pc[:, 1, :]), reads=["pc1", "pcb"], writes=["sc0"])
        cur = 0
        dd = 1
        while dd < 64:
            a, b = sc[cur], sc[1 - cur]
            an, bn = f"sc{cur}", f"sc{1 - cur}"
            P.op("dve", lambda e, a=a, b=b, dd=dd: e.tensor_copy(out=b[:, 0:dd], in_=a[:, 0:dd]), reads=[an], writes=[bn])
            P.op("dve", lambda e, a=a, b=b, dd=dd: e.tensor_tensor(out=b[:, dd:64], in0=a[:, dd:64], in1=a[:, 0:64 - dd], op=ALU.add),
                 reads=[an, bn], writes=[bn])
            cur = 1 - cur
            dd *= 2
        inc = sc[cur]; incn = f"sc{cur}"
        P.op("dve", lambda e, inc=inc: e.tensor_tensor(out=ex[:], in0=inc[:], in1=pc[:, 1, :], op=ALU.subtract), reads=[incn, "pc1", "pcb"], writes=["ex"])
        P.op("dve", lambda e: e.tensor_tensor(out=ccol[:], in0=pc[:, 0, :], in1=ex[:], op=ALU.add), reads=["pc0", "pcb", "ex"], writes=["ccol"])
        for qt in range(32):
            P.op("dve", lambda e, qt=qt: e.tensor_scalar(out=G[:, qt, :], in0=ccol[:], scalar1=-1.0, scalar2=ex[:, 2 * qt:2 * qt + 1],
                                                         op0=ALU.mult, op1=ALU.add), reads=["ccol", "ex"], writes=[("G", qt)])

    def run_maps(maps, V, vname, dv, epilogue, nq=32):
        nm = len(maps)
        for qt in range(nq):
            qtiles = []
            for (ki, qi) in maps:
                s = state["q"] % 4; state["q"] += 1
                P.dma("sp", lambda e, s=s, qi=qi, qt=qt: e.dma_start(out=qt_[s][:], in_=qkT[qi, :, qt * 256:(qt + 1) * 256]), writes=[f"qt{s}"])
                qtiles.append(s)
            nkb = 2 * qt + 2
            steps = [(kb, m) for kb in range(nkb) for m in range(nm)]
            sslot = {}
            def issue_S(idx):
                kb, m = steps[idx]
                s = state["s"] % 3; state["s"] += 1
                sslot[idx] = s
                ki = maps[m][0]
                lo = 128 if kb == nkb - 1 else 0
                qb = qtiles[m]
                P.op("pe", lambda e, s=s, ki=ki, kb=kb, qb=qb, lo=lo: e.matmul(ps_s[:, s, lo:256], lhsT=KT[:, ki, kb * 128:(kb + 1) * 128],
                                                                              rhs=qt_[qb][:, lo:256], start=True, stop=True),
                     reads=[("KT", ki), f"qt{qb}"], writes=[("ps_s", s)], after=[state["f32mm"]] if "f32mm" in state else [])
            LA = 2
            for idx in range(min(LA, len(steps))):
                issue_S(idx)
            for idx, (kb, m) in enumerate(steps):
                if idx + LA < len(steps):
                    issue_S(idx + LA)
                s = sslot[idx]
                p = state["p"] % 4; state["p"] += 1
                lo = 128 if kb == nkb - 1 else 0
                P.op("act", lambda e, s=s, p=p, kb=kb, qt=qt, lo=lo: e.activation(out=pt[p][:, lo:256], in_=ps_s[:, s, lo:256], func=AF.Exp,
                                                                                bias=G[:, qt, kb:kb + 1], scale=SCALE),
                     reads=[("ps_s", s), ("G", qt)], writes=[f"pt{p}"])
                if kb >= nkb - 2:
                    j = kb - (nkb - 2)
                    P.op("dve", lambda e, p=p, j=j: e.tensor_tensor(out=pt[p][:, j * 128:(j + 1) * 128], in0=pt[p][:, j * 128:(j + 1) * 128],
                                                                    in1=trib[:], op=ALU.mult), reads=[f"pt{p}", "trib"], writes=[f"pt{p}"])
                for j in range(2):
                    if j == 0 and kb == nkb - 1:
                        continue
                    last = (kb == nkb - 2) if j == 0 else (kb == nkb - 1)
                    r = m * 2 + j
                    P.op("pe", lambda e, p=p, j=j, kb=kb, r=r, last=last: e.matmul(po[:, r, 0:dv + 1], lhsT=pt[p][:, j * 128:(j + 1) * 128],
                                                                                   rhs=V[:, kb, :], start=(kb == 0), stop=last),
                         reads=[f"pt{p}", (vname, kb // 16), vname + "o"], writes=[("po", r)])
            epilogue(qt)

    def fox_epilogue(h):
        def ep(qt):
            o = state["o"] % 2; state["o"] += 1
            for j in range(2):
                r = state["r"] % 4; state["r"] += 1
                P.op("dve", lambda e, r=r, j=j: e.reciprocal(out=rec[r][:], in_=po[:, j, 128:129]), reads=[("po", j)], writes=[f"rec{r}"])
                P.op("dve", lambda e, r=r, j=j, o=o: e.tensor_scalar(out=ost[o][:, j, 0:128], in0=po[:, j, 0:128], scalar1=rec[r][:], scalar2=None,
                                                                     op0=ALU.mult), reads=[("po", j), f"rec{r}"], writes=[f"ost{o}"])
            P.dma("pool", lambda e, o=o, qt=qt: e.dma_start(
                out=o_out[qt * 256:(qt + 1) * 256, h * 128:(h + 1) * 128].rearrange("(j p) c -> p j c", p=128), in_=ost[o][:, :, 0:128]),
                reads=[f"ost{o}"], writes=[("o_out", h, qt)], is_output=out_is_final)
        return ep

    def diff_epilogue(qt):
        o = state["o"] % 2; state["o"] += 1
        for j in range(2):
            r0 = state["r"] % 4; state["r"] += 1
            r1 = state["r"] % 4; state["r"] += 1
            P.op("dve", lambda e, r0=r0, j=j: e.reciprocal(out=rec[r0][:], in_=po[:, j, 256:257]), reads=[("po", j)], writes=[f"rec{r0}"])
            P.op("dve", lambda e, r1=r1, j=j: e.reciprocal(out=rec[r1][:], in_=po[:, 2 + j, 256:257]), reads=[("po", 2 + j)], writes=[f"rec{r1}"])
            P.op("dve", lambda e, r1=r1: e.tensor_tensor(out=rec[r1][:], in0=rec[r1][:], in1=neglam[:], op=ALU.mult), reads=[f"rec{r1}", "neglam"], writes=[f"rec{r1}"])
            P.op("dve", lambda e, r0=r0, j=j: e.tensor_scalar(out=t0[j][:], in0=po[:, j, 0:256], scalar1=rec[r0][:], scalar2=None, op0=ALU.mult),
                 reads=[("po", j), f"rec{r0}"], writes=[f"t0{j}"])
            P.op("dve", lambda e, r1=r1, j=j: e.scalar_tensor_tensor(out=t1[j][:], in0=po[:, 2 + j, 0:256], scalar=rec[r1][:], in1=t0[j][:],
                                                                     op0=ALU.mult, op1=ALU.add), reads=[("po", 2 + j), f"rec{r1}", f"t0{j}"], writes=[f"t1{j}"])
            P.op("act", lambda e, j=j: e.activation(out=junk2[:], in_=t1[j][:], func=AF.Square, accum_out=ssd[j][:]), reads=[f"t1{j}"], writes=["junk2", f"ssd{j}"])
            P.op("act", lambda e, j=j: e.activation(out=ssd[j][:], in_=ssd[j][:], func=AF.Sqrt, bias=1e-5, scale=1.0 / 256), reads=[f"ssd{j}"], writes=[f"ssd{j}"])
            P.op("dve", lambda e, j=j: e.reciprocal(out=ssd[j][:], in_=ssd[j][:]), reads=[f"ssd{j}"], writes=[f"ssd{j}"])
            P.op("dve", lambda e, j=j, o=o: e.scalar_tensor_tensor(out=ost[o][:, j, :], in0=t1[j][:], scalar=ssd[j][:], in1=gs[:], op0=ALU.mult, op1=ALU.mult),
                 reads=[f"t1{j}", f"ssd{j}", "gs"], writes=[f"ost{o}"])
        P.dma("pool", lambda e, o=o, qt=qt: e.dma_start(
            out=o_out[qt * 256:(qt + 1) * 256, 256:512].rearrange("(j p) c -> p j c", p=128), in_=ost[o][:]),
            reads=[f"ost{o}"], writes=[("o_out", 2, qt)], is_output=out_is_final)

    Gall = [("G", qt) for qt in range(32)]
    for h in do_fox:
        fox_table(h)
        run_maps([(h, h)], Vf[h], f"Vf{h}", 128, fox_epilogue(h), nq=nq)
    if do_diff:
        P.dma("sp", lambda e: e.dma_start(out=G[:].rearrange("p a b -> p (a b)"), in_=alibi_d[:, :]), writes=Gall)
    if dbg is not None:
        P.dma("sp", lambda e: e.dma_start(out=dbg[:, 0:2048], in_=G[:].rearrange("p a b -> p (a b)")), reads=Gall, writes=["dbg0"], is_output=True)
        P.dma("sp", lambda e: e.dma_start(out=dbg[:, 2048:2112], in_=ccol[:]), reads=["ccol"], writes=["dbg1"], is_output=True)
        P.dma("sp", lambda e: e.dma_start(out=dbg[:, 2112:2176], in_=ex[:]), reads=["ex"], writes=["dbg2"], is_output=True)
        P.dma("sp", lambda e: e.dma_start(out=dbg[:, 2176:2240], in_=lf[:]), reads=["lf"], writes=["dbg3"], is_output=True)
    if do_diff:
        run_maps([(2, 2), (3, 3)], Vd, "Vd", 256, diff_epilogue, nq=nq)
    P.end_phase(final=out_is_final)


def build_l1(debug=False, only_proj=False):
    nc = bass.Bass("TRN2", target_bir_lowering=False)
    x = nc.dram_tensor("x", [S, D], F32, kind="ExternalInput").ap()
    wc = nc.dram_tensor("wc", [D, NWC], F32, kind="ExternalInput").ap()
    gcol = nc.dram_tensor("gcol", [1, D], F32, kind="ExternalInput").ap()
    ident = nc.dram_tensor("ident", [128, 128], BF16, kind="ExternalInput").ap()
    fbias = nc.dram_tensor("fbias", [1, 2], F32, kind="ExternalInput").ap()
    alibi = nc.dram_tensor("alibi", [128, 2048], F32, kind="ExternalInput").ap()
    tri = nc.dram_tensor("tri", [128, 128], F32, kind="ExternalInput").ap()
    ones = nc.dram_tensor("ones", [128, 128], F32, kind="ExternalInput").ap()
    lamv = nc.dram_tensor("lamv", [1, 4, 128], F32, kind="ExternalInput").ap()
    sublng = nc.dram_tensor("sublng", [1, 256], F32, kind="ExternalInput").ap()
    kind = "ExternalOutput" if debug else "Internal"
    qkT = nc.dram_tensor("qkT", [8, 128, S], BF16, kind=kind).ap()
    vdr = nc.dram_tensor("vdr", [S, 512], BF16, kind=kind).ap()
    ffd = nc.dram_tensor("ffd", [128, 128], F32, kind=kind).ap()
    o = nc.dram_tensor("o", [S, 512], BF16, kind="ExternalOutput").ap()
    P = Prog(nc)
    phase_proj(P, nc, x, wc, gcol, ident, qkT, vdr, ffd)
    if only_proj:
        P.es.close()
        return nc
    phase_attn(P, nc, qkT, vdr, ffd, fbias, alibi, tri, ones, lamv, sublng, o, True)
    return nc


def l1_inputs(inp):
    x = inp["x"]
    w_in = inp["w_in"][0]
    offs = np.cumsum([0, 1024, 1024, 1024, 8, 1024, 1024, 1024])
    g = inp["attn_norm_g"][0]
    gcol = np.ascontiguousarray(g[None, :])
    ident = np.eye(128, dtype=np.float32).astype(NPBF)
    tri = np.triu(np.ones((128, 128), np.float32))
    ones = np.ones((128, 128), np.float32)
    lamv = np.stack([inp["lambda_q1"][0], inp["lambda_k1"][0], inp["lambda_q2"][0], inp["lambda_k2"][0]])[None]
    slopes = 2.0 ** (-8.0 * np.arange(1, 5) / 4)
    p = np.arange(128, dtype=np.float32)[:, None, None]
    qt = np.arange(32, dtype=np.float32)[None, :, None]
    kb = np.arange(64, dtype=np.float32)[None, None, :]
    rel = kb * 128 + p - qt * 256
    maps = []
    for c in range(NCORES):
        b, j = c // 4, c % 4
        cols = np.concatenate([
            np.arange(offs[0] + 2 * j * 128, offs[0] + (2 * j + 2) * 128), np.arange(offs[4] + j * 256, offs[4] + (j + 1) * 256),
            np.arange(offs[1] + 2 * j * 128, offs[1] + (2 * j + 2) * 128), np.arange(offs[5] + j * 256, offs[5] + (j + 1) * 256),
            np.arange(offs[2] + 2 * j * 128, offs[2] + (2 * j + 2) * 128), np.arange(offs[6] + j * 256, offs[6] + (j + 1) * 256),
            np.arange(offs[3] + 2 * j, offs[3] + 2 * j + 2)])
        maps.append({
            "x": np.ascontiguousarray(x[b]),
            "wc": np.ascontiguousarray(w_in[:, cols]),
            "gcol": gcol, "ident": ident,
            "fbias": np.ascontiguousarray(inp["forget_bias"][0][2 * j:2 * j + 2][None]),
            "alibi": np.ascontiguousarray((np.float32(slopes[j]) * rel).astype(np.float32).reshape(128, 2048)),
            "tri": tri, "ones": ones, "lamv": np.ascontiguousarray(lamv.astype(np.float32)),
            "sublng": np.ascontiguousarray(inp["diff_subln_g"]),
        })
    return maps


def build_attn_only(nq=32, do_fox=(0, 1), do_diff=True):
    nc = bass.Bass("TRN2", target_bir_lowering=False)
    fbias = nc.dram_tensor("fbias", [1, 2], F32, kind="ExternalInput").ap()
    alibi = nc.dram_tensor("alibi", [128, 2048], F32, kind="ExternalInput").ap()
    tri = nc.dram_tensor("tri", [128, 128], F32, kind="ExternalInput").ap()
    ones = nc.dram_tensor("ones", [128, 128], F32, kind="ExternalInput").ap()
    lamv = nc.dram_tensor("lamv", [1, 4, 128], F32, kind="ExternalInput").ap()
    sublng = nc.dram_tensor("sublng", [1, 256], F32, kind="ExternalInput").ap()
    qkT = nc.dram_tensor("qkT", [8, 128, S], BF16, kind="ExternalInput").ap()
    vdr = nc.dram_tensor("vdr", [S, 512], BF16, kind="ExternalInput").ap()
    ffd = nc.dram_tensor("ffd", [128, 128], F32, kind="ExternalInput").ap()
    o = nc.dram_tensor("o", [S, 512], BF16, kind="ExternalOutput").ap()
    dbg = nc.dram_tensor("dbg", [128, 2240], F32, kind="ExternalOutput").ap()
    P = Prog(nc)
    phase_attn(P, nc, qkT, vdr, ffd, fbias, alibi, tri, ones, lamv, sublng, o, True, nq=nq, do_fox=do_fox, do_diff=do_diff, dbg=dbg)
    return nc


NT = 2048


def build_l2a(with_router=False):
    nc = bass.Bass("TRN2", target_bir_lowering=False)
    oT = nc.dram_tensor("oT", [16, 128, NT], BF16, kind="ExternalInput").ap()
    xs = nc.dram_tensor("xs", [NT, D], F32, kind="ExternalInput").ap()
    wo = nc.dram_tensor("wo", [D, D], F32, kind="ExternalInput").ap()
    gf = nc.dram_tensor("gf", [1, D], F32, kind="ExternalInput").ap()
    h_o = nc.dram_tensor("h", [NT, D], F32, kind="ExternalOutput").ap()
    hnf_o = nc.dram_tensor("hnf", [NT, D], F32, kind="ExternalOutput").ap()
    hnb_o = nc.dram_tensor("hnb", [NT, D], BF16, kind="ExternalOutput").ap()
    if with_router:
        wr = nc.dram_tensor("wr", [D, 72], F32, kind="ExternalInput").ap()
        br = nc.dram_tensor("br", [1, 72], F32, kind="ExternalInput").ap()
        io8 = nc.dram_tensor("io8", [1, 8], F32, kind="ExternalInput").ap()
        idf = nc.dram_tensor("identf", [128, 128], F32, kind="ExternalInput").ap()
        ri_o = nc.dram_tensor("ri", [NT, 8], F32, kind="ExternalOutput").ap()
    P = Prog(nc)
    P.begin_phase()
    wob = P.sb("wob", [128, 16, D], BF16)
    g = P.sb("g", [128, D], F32)
    oTt = [P.sb(f"oTt{i}", [128, 16, 128], BF16) for i in range(2)]
    xt = [P.sb(f"xt{i}", [128, D], F32) for i in range(2)]
    ht = [P.sb(f"ht{i}", [128, D], F32) for i in range(2)]
    hn = [P.sb(f"hn{i}", [128, D], F32) for i in range(2)]
    hb = [P.sb(f"hb{i}", [128, D], BF16) for i in range(2)]
    junk = P.sb("junk", [128, D], BF16)
    ss = [P.sb(f"ss{i}", [128, 1], F32) for i in range(2)]
    nring = 5 if with_router else 8
    ph = P.ps("ph", [128, nring, 512], F32)
    ring = [0]
    if with_router:
        wrs = P.sb("wrs", [128, 16, 72], F32); brs = P.sb("brs", [128, 72], F32); iota = P.sb("iota", [128, 8], F32)
        identf = P.sb("identf", [128, 128], F32)
        hT32 = P.sb("hT32", [128, 16, 128], F32)
        ptf = P.ps("ptf", [128, 4, 128], F32)
        plog = P.ps("plog", [128, 2, 512], F32)
        P.dma("sp", lambda e: e.dma_start(out=wrs[:], in_=wr.rearrange("(c p) n -> p c n", p=128)), writes=["wrs"])
        P.dma("sp", lambda e: e.dma_start(out=brs[:], in_=br[0:1, :].broadcast_to([128, 72])), writes=["brs"])
        P.dma("sp", lambda e: e.dma_start(out=iota[:], in_=io8[0:1, :].broadcast_to([128, 8])), writes=["iota"])
        P.dma("sp", lambda e: e.dma_start(out=identf[:], in_=idf[:, :]), writes=["identf"])
    P.dma("sp", lambda e: e.dma_start(out=g[:], in_=gf[0:1, :].broadcast_to([128, D])), writes=["g"])
    for c in range(16):
        P.dma("pool", lambda e, c=c: e.dma_start(out=wob[:, c, :], in_=wo[c * 128:(c + 1) * 128, :]), writes=[("wob", c)])
    for ti in range(NT // 128):
        b = ti % 2
        P.dma("sp", lambda e, b=b, ti=ti: e.dma_start(out=oTt[b][:], in_=oT[:, :, ti * 128:(ti + 1) * 128].rearrange("c p t -> p c t")), writes=[f"oTt{b}"])
        P.dma("sp", lambda e, b=b, ti=ti: e.dma_start(out=xt[b][:], in_=xs[ti * 128:(ti + 1) * 128, :]), writes=[f"xt{b}"])
        for cc in range(4):
            for c in range(16):
                if c == 0:
                    pbk = ring[0] % nring; ring[0] += 1
                P.op("pe", lambda e, b=b, cc=cc, c=c, pbk=pbk: e.matmul(ph[:, pbk, :], lhsT=oTt[b][:, c, :], rhs=wob[:, c, cc * 512:(cc + 1) * 512],
                                                                        start=(c == 0), stop=(c == 15)), reads=[("wob", c), f"oTt{b}"], writes=[("ph", pbk)])
            P.op("dve", lambda e, b=b, cc=cc, pbk=pbk: e.tensor_tensor(out=ht[b][:, cc * 512:(cc + 1) * 512], in0=ph[:, pbk, :], in1=xt[b][:, cc * 512:(cc + 1) * 512], op=ALU.add),
                 reads=[("ph", pbk), f"xt{b}"], writes=[(f"ht{b}", cc)])
        hall = [(f"ht{b}", cc) for cc in range(4)]
        P.dma("pool", lambda e, b=b, ti=ti: e.dma_start(out=h_o[ti * 128:(ti + 1) * 128, :], in_=ht[b][:]), reads=hall, writes=[("h_o", ti)], is_output=True)
        P.op("act", lambda e, b=b: e.activation(out=junk[:], in_=ht[b][:], func=AF.Square, accum_out=ss[b][:]), reads=hall, writes=["junk", f"ss{b}"])
        P.op("act", lambda e, b=b: e.activation(out=ss[b][:], in_=ss[b][:], func=AF.Sqrt, bias=1e-6, scale=1.0 / D), reads=[f"ss{b}"], writes=[f"ss{b}"])
        P.op("dve", lambda e, b=b: e.reciprocal(out=ss[b][:], in_=ss[b][:]), reads=[f"ss{b}"], writes=[f"ss{b}"])
        P.op("dve", lambda e, b=b: e.scalar_tensor_tensor(out=hn[b][:], in0=ht[b][:], scalar=ss[b][:], in1=g[:], op0=ALU.mult, op1=ALU.mult),
             reads=hall + [f"ss{b}", "g"], writes=[f"hn{b}"])
        P.op("act", lambda e, b=b: e.copy(out=hb[b][:], in_=hn[b][:]), reads=[f"hn{b}"], writes=[f"hb{b}"])
        P.dma("pool", lambda e, b=b, ti=ti: e.dma_start(out=hnf_o[ti * 128:(ti + 1) * 128, :], in_=hn[b][:]), reads=[f"hn{b}"], writes=[("hnf_o", ti)], is_output=True)
        P.dma("pool", lambda e, b=b, ti=ti: e.dma_start(out=hnb_o[ti * 128:(ti + 1) * 128, :], in_=hb[b][:]), reads=[f"hb{b}"], writes=[("hnb_o", ti)], is_output=True)
        if with_router:
            for q4 in range(4):
                for i4 in range(4):
                    c = q4 * 4 + i4
                    P.op("pe", lambda e, b=b, c=c, i4=i4: e.transpose(ptf[:, i4, :], hn[b][:, c * 128:(c + 1) * 128], identf[:]),
                         reads=[f"hn{b}", "identf"], writes=["ptf"])
                P.op("act", lambda e, q4=q4: e.copy(out=hT32[:, q4 * 4:(q4 + 1) * 4, :], in_=ptf[:]), reads=["ptf"], writes=[("hT32", q4)])
            for c in range(16):
                P.op("pe", lambda e, b=b, c=c: e.matmul(plog[:, b, 0:72], lhsT=hT32[:, c, :], rhs=wrs[:, c, :], start=(c == 0), stop=(c == 15)),
                     reads=[("hT32", q4) for q4 in range(4)] + ["wrs"], writes=[("plog", b)])
            emit_router(P, ti, b, plog, brs, iota, ri_o)
    P.end_phase(final=True)
    return nc


def emit_router(P, ti, b, plog, brs, iota, ri_o):
    if True:
        T = {}
        def sbt(nm, w):
            T[nm] = P.sb(f"{nm}_{ti}", [128, w], F32)
            return T[nm]
        lg = sbt("lg", 72); goh = sbt("goh", 8); gex = sbt("gex", 8); t8 = sbt("t8", 8); sel = sbt("sel", 8)
        oh1 = sbt("oh1", 8); sel2 = sbt("sel2", 8); oh2 = sbt("oh2", 8); s1 = sbt("s1", 8); ri = sbt("ri", 8)
        rn = lambda k: f"{k}_{ti}"
        def dv(fn, reads, writes):
            P.op("dve", fn, reads=[rn(r) if isinstance(r, str) and not r.startswith("@") else (r[1:] if isinstance(r, str) else r) for r in reads],
                 writes=[rn(w) for w in writes])
        dv(lambda e, b=b, lg=lg: e.tensor_tensor(out=lg[:], in0=plog[:, b, 0:72], in1=brs[:], op=ALU.add), [("plog", b), "@brs"], ["lg"])
        dv(lambda e, lg=lg, s1=s1: e.tensor_reduce(out=s1[:, 0:1], in_=lg[:, 0:8], axis=AX.X, op=ALU.max), ["lg"], ["gm"])
        dv(lambda e, lg=lg, s1=s1, goh=goh: e.tensor_scalar(out=goh[:], in0=lg[:, 0:8], scalar1=s1[:, 0:1], scalar2=None, op0=ALU.is_equal), ["lg", "gm"], ["goh"])
        dv(lambda e, s1=s1: e.tensor_scalar(out=s1[:, 1:2], in0=s1[:, 0:1], scalar1=-1.0, scalar2=None, op0=ALU.mult), ["gm"], ["ngm"])
        P.op("act", lambda e, lg=lg, s1=s1, gex=gex: e.activation(out=gex[:], in_=lg[:, 0:8], func=AF.Exp, bias=s1[:, 1:2], scale=1.0, accum_out=s1[:, 2:3]),
             reads=[rn("lg"), rn("ngm")], writes=[rn("gex"), rn("gsum")])
        dv(lambda e, s1=s1: e.reciprocal(out=s1[:, 3:4], in_=s1[:, 2:3]), ["gsum"], ["gprob"])
        dv(lambda e, goh=goh, t8=t8: e.tensor_tensor(out=t8[:], in0=goh[:], in1=iota[:], op=ALU.mult), ["goh", "@iota"], ["t8"])
        dv(lambda e, t8=t8, ri=ri: e.tensor_reduce(out=ri[:, 0:1], in_=t8[:], axis=AX.X, op=ALU.add), ["t8"], ["ri0"])
        for gi in range(8):
            if gi == 0:
                dv(lambda e, lg=lg, goh=goh, sel=sel: e.tensor_scalar(out=sel[:], in0=lg[:, 8:16], scalar1=goh[:, 0:1], scalar2=None, op0=ALU.mult), ["lg", "goh"], ["sel"])
            else:
                dv(lambda e, lg=lg, goh=goh, sel=sel, gi=gi: e.scalar_tensor_tensor(out=sel[:], in0=lg[:, 8 + gi * 8:16 + gi * 8], scalar=goh[:, gi:gi + 1], in1=sel[:],
                                                                                  op0=ALU.mult, op1=ALU.add), ["lg", "goh", "sel"], ["sel"])
        dv(lambda e, sel=sel, s1=s1: e.tensor_reduce(out=s1[:, 4:5], in_=sel[:], axis=AX.X, op=ALU.max), ["sel"], ["m1"])
        dv(lambda e, sel=sel, s1=s1, oh1=oh1: e.tensor_scalar(out=oh1[:], in0=sel[:], scalar1=s1[:, 4:5], scalar2=None, op0=ALU.is_equal), ["sel", "m1"], ["oh1"])
        dv(lambda e, sel=sel, oh1=oh1, sel2=sel2: e.scalar_tensor_tensor(out=sel2[:], in0=oh1[:], scalar=-1e30, in1=sel[:], op0=ALU.mult, op1=ALU.add), ["sel", "oh1"], ["sel2"])
        dv(lambda e, sel2=sel2, s1=s1: e.tensor_reduce(out=s1[:, 5:6], in_=sel2[:], axis=AX.X, op=ALU.max), ["sel2"], ["m2"])
        dv(lambda e, sel2=sel2, s1=s1, oh2=oh2: e.tensor_scalar(out=oh2[:], in0=sel2[:], scalar1=s1[:, 5:6], scalar2=None, op0=ALU.is_equal), ["sel2", "m2"], ["oh2"])
        dv(lambda e, oh1=oh1, t8=t8: e.tensor_tensor(out=t8[:], in0=oh1[:], in1=iota[:], op=ALU.mult), ["oh1", "@iota", "ri0"], ["t8"])
        dv(lambda e, t8=t8, ri=ri: e.tensor_reduce(out=ri[:, 1:2], in_=t8[:], axis=AX.X, op=ALU.add), ["t8"], ["ri1"])
        dv(lambda e, oh2=oh2, t8=t8: e.tensor_tensor(out=t8[:], in0=oh2[:], in1=iota[:], op=ALU.mult), ["oh2", "@iota", "ri1"], ["t8"])
        dv(lambda e, t8=t8, ri=ri: e.tensor_reduce(out=ri[:, 2:3], in_=t8[:], axis=AX.X, op=ALU.add), ["t8"], ["ri2"])
        dv(lambda e, s1=s1: e.tensor_tensor(out=s1[:, 6:7], in0=s1[:, 5:6], in1=s1[:, 4:5], op=ALU.subtract), ["m1", "m2"], ["dd"])
        P.op("act", lambda e, s1=s1: e.activation(out=s1[:, 6:7], in_=s1[:, 6:7], func=AF.Exp), reads=[rn("dd")], writes=[rn("dd")])
        dv(lambda e, s1=s1: e.tensor_scalar(out=s1[:, 7:8], in0=s1[:, 6:7], scalar1=1.0, scalar2=None, op0=ALU.add), ["dd"], ["w1"])
        dv(lambda e, s1=s1: e.reciprocal(out=s1[:, 7:8], in_=s1[:, 7:8]), ["w1"], ["w1"])
        dv(lambda e, s1=s1, ri=ri: e.tensor_tensor(out=ri[:, 3:4], in0=s1[:, 7:8], in1=s1[:, 3:4], op=ALU.mult), ["w1", "gprob"], ["ri3"])
        dv(lambda e, s1=s1, ri=ri: e.tensor_tensor(out=ri[:, 4:5], in0=ri[:, 3:4], in1=s1[:, 6:7], op=ALU.mult), ["ri3", "dd"], ["ri4"])
        dv(lambda e, ri=ri: e.memset(ri[:, 5:8], 0.0), [], ["ri5"])
        P.dma("sp", lambda e, ri=ri, ti=ti: e.dma_start(out=ri_o[ti * 128:(ti + 1) * 128, :], in_=ri[:]),
              reads=[rn(k) for k in ("ri0", "ri1", "ri2", "ri3", "ri4", "ri5")], writes=[("ri_o", ti)], is_output=True)


def build_l2b():
    nc = bass.Bass("TRN2", target_bir_lowering=False)
    hT = nc.dram_tensor("hT", [16, 128, NT], F32, kind="ExternalInput").ap()
    wr = nc.dram_tensor("wr", [D, 72], F32, kind="ExternalInput").ap()
    br = nc.dram_tensor("br", [1, 72], F32, kind="ExternalInput").ap()
    io8 = nc.dram_tensor("io8", [1, 8], F32, kind="ExternalInput").ap()
    ri_o = nc.dram_tensor("ri", [NT, 8], F32, kind="ExternalOutput").ap()
    P = Prog(nc)
    P.begin_phase()
    wrs = P.sb("wrs", [128, 16, 72], F32)
    brs = P.sb("brs", [128, 72], F32)
    iota = P.sb("iota", [128, 8], F32)
    hTt = [P.sb(f"hTt{i}", [128, 16, 128], F32) for i in range(2)]
    plog = P.ps("plog", [128, 2, 512], F32)
    P.dma("sp", lambda e: e.dma_start(out=wrs[:], in_=wr.rearrange("(c p) n -> p c n", p=128)), writes=["wrs"])
    P.dma("sp", lambda e: e.dma_start(out=brs[:], in_=br[0:1, :].broadcast_to([128, 72])), writes=["brs"])
    P.dma("sp", lambda e: e.dma_start(out=iota[:], in_=io8[0:1, :].broadcast_to([128, 8])), writes=["iota"])
    names = ["lg", "goh", "gex", "t8", "sel", "oh1", "sel2", "oh2"]
    for ti in range(NT // 128):
        b = ti % 2
        P.dma("sp", lambda e, b=b, ti=ti: e.dma_start(out=hTt[b][:], in_=hT[:, :, ti * 128:(ti + 1) * 128].rearrange("c p t -> p c t")), writes=[f"hTt{b}"])
        for c in range(16):
            P.op("pe", lambda e, b=b, c=c: e.matmul(plog[:, b, 0:72], lhsT=hTt[b][:, c, :], rhs=wrs[:, c, :], start=(c == 0), stop=(c == 15)),
                 reads=[f"hTt{b}", "wrs"], writes=[("plog", b)])
        emit_router(P, ti, b, plog, brs, iota, ri_o)
    P.end_phase(final=True)
    return nc


def build_l3(cap):
    ncb = cap // 128
    nc = bass.Bass("TRN2", target_bir_lowering=False)
    XT = nc.dram_tensor("XT", [8, 16, 128, cap], BF16, kind="ExternalInput").ap()
    gcol = nc.dram_tensor("gcol", [128, 8 * ncb], F32, kind="ExternalInput").ap()
    wg = nc.dram_tensor("wg", [8, D, 1024], F32, kind="ExternalInput").ap()
    wu = nc.dram_tensor("wu", [8, D, 1024], F32, kind="ExternalInput").ap()
    wd = nc.dram_tensor("wd", [8, 1024, D], F32, kind="ExternalInput").ap()
    Y = nc.dram_tensor("Y", [8, cap, D], F32, kind="ExternalOutput").ap()
    P = Prog(nc)
    P.begin_phase()
    wbuf = [P.sb(f"wbuf{i}", [128, 16384], BF16) for i in range(4)]
    nxb = 2 if cap <= 640 else 1
    xT = [P.sb(f"xT{i}", [128, 16, cap], BF16) for i in range(nxb)]
    gc = P.sb("gc", [128, 8 * ncb], F32)
    nch = (cap + 511) // 512
    cw = ((ncb + nch - 1) // nch) * 128
    chunks = [(r0, min(cw, cap - r0)) for r0 in range(0, cap, cw)]
    pbi = [0]
    sg = [P.sb(f"sg{i}", [128, cw], F32) for i in range(2)]
    hdn = P.sb("hdn", [128, 8, cap], BF16)
    ys = [P.sb(f"ys{i}", [128, D], F32) for i in range(2)]
    pg = P.ps("pg", [128, 2, 512], F32)
    pu = P.ps("pu", [128, 2, 512], F32)
    py = P.ps("py", [128, 2, 512], F32)
    P.dma("sp", lambda e: e.dma_start(out=gc[:], in_=gcol[:, :]), writes=["gc"])
    wi = 0
    yi = 0
    for ex in range(8):
        xb = ex % nxb
        P.dma("sp", lambda e, xb=xb, ex=ex: e.dma_start(out=xT[xb][:], in_=XT[ex].rearrange("c p r -> p c r")), writes=[f"xT{xb}"])
        bufs = []
        for (w, kk) in ((wg, 16), (wu, 16), (wd, 8)):
            bi = wi % 4; wi += 1
            for half in range(2):
                k2 = kk // 2
                def ld(e, bi=bi, w=w, ex=ex, kk=kk, half=half, k2=k2):
                    ncol = 16384 // kk
                    dst = wbuf[bi][:].rearrange("p (c f) -> p c f", c=kk)[:, half * k2:(half + 1) * k2, :]
                    src = w[ex].rearrange("(c p) f -> p c f", p=128)[:, half * k2:(half + 1) * k2, :]
                    return e.dma_start(out=dst, in_=src)
                P.dma("pool", ld, writes=[(f"wbuf{bi}", half)])
            bufs.append(bi)
        bg, bu, bd = bufs
        wgv = wbuf[bg][:].rearrange("p (c f) -> p c f", c=16)
        wuv = wbuf[bu][:].rearrange("p (c f) -> p c f", c=16)
        wdv = wbuf[bd][:].rearrange("p (c f) -> p c f", c=8)
        for (r0, rw) in chunks:
            for f in range(8):
                pb = pbi[0] % 2; pbi[0] += 1
                for c in range(16):
                    P.op("pe", lambda e, pb=pb, c=c, f=f, wgv=wgv, xb=xb, r0=r0, rw=rw: e.matmul(pg[:, pb, 0:rw], lhsT=wgv[:, c, f * 128:(f + 1) * 128],
                                                                                               rhs=xT[xb][:, c, r0:r0 + rw], start=(c == 0), stop=(c == 15)),
                         reads=[(f"wbuf{bg}", 0), (f"wbuf{bg}", 1), f"xT{xb}"], writes=[("pg", pb)])
                for c in range(16):
                    P.op("pe", lambda e, pb=pb, c=c, f=f, wuv=wuv, xb=xb, r0=r0, rw=rw: e.matmul(pu[:, pb, 0:rw], lhsT=wuv[:, c, f * 128:(f + 1) * 128],
                                                                                               rhs=xT[xb][:, c, r0:r0 + rw], start=(c == 0), stop=(c == 15)),
                         reads=[(f"wbuf{bu}", 0), (f"wbuf{bu}", 1), f"xT{xb}"], writes=[("pu", pb)])
                P.op("act", lambda e, pb=pb, rw=rw: e.activation(out=sg[pb][:, 0:rw], in_=pg[:, pb, 0:rw], func=AF.Silu), reads=[("pg", pb)], writes=[f"sg{pb}"])
                P.op("dve", lambda e, pb=pb, f=f, r0=r0, rw=rw: e.tensor_tensor(out=hdn[:, f, r0:r0 + rw], in0=sg[pb][:, 0:rw], in1=pu[:, pb, 0:rw], op=ALU.mult),
                     reads=[f"sg{pb}", ("pu", pb)], writes=[("hdn", f, r0)])
        hall = [("hdn", f, r0) for f in range(8) for (r0, rw) in chunks]
        for rb in range(ncb):
            yb = yi % 2; yi += 1
            for cc in range(4):
                pb = cc % 2
                for f in range(8):
                    P.op("pe", lambda e, pb=pb, f=f, rb=rb, cc=cc, wdv=wdv: e.matmul(py[:, pb, :], lhsT=hdn[:, f, rb * 128:(rb + 1) * 128],
                                                                                     rhs=wdv[:, f, cc * 512:(cc + 1) * 512], start=(f == 0), stop=(f == 7)),
                         reads=hall + [(f"wbuf{bd}", 0), (f"wbuf{bd}", 1)], writes=[("py", pb)])
                gi = ex * ncb + rb
                P.op("dve", lambda e, pb=pb, yb=yb, cc=cc, gi=gi: e.tensor_scalar(out=ys[yb][:, cc * 512:(cc + 1) * 512], in0=py[:, pb, :], scalar1=gc[:, gi:gi + 1],
                                                                                  scalar2=None, op0=ALU.mult), reads=[("py", pb), "gc"], writes=[(f"ys{yb}", cc)])
            P.dma("sp", lambda e, yb=yb, ex=ex, rb=rb: e.dma_start(out=Y[ex, rb * 128:(rb + 1) * 128, :], in_=ys[yb][:]),
                  reads=[(f"ys{yb}", cc) for cc in range(4)], writes=[("Y", ex, rb)], is_output=True)
    P.end_phase(final=True)
    return nc


def build_l4():
    nc = bass.Bass("TRN2", target_bir_lowering=False)
    h = nc.dram_tensor("h", [NT, D], F32, kind="ExternalInput").ap()
    y1 = nc.dram_tensor("y1", [NT, D], F32, kind="ExternalInput").ap()
    y2 = nc.dram_tensor("y2", [NT, D], F32, kind="ExternalInput").ap()
    gf = nc.dram_tensor("gf", [1, D], F32, kind="ExternalInput").ap()
    out = nc.dram_tensor("out", [NT, D], F32, kind="ExternalOutput").ap()
    P = Prog(nc)
    P.begin_phase()
    g = P.sb("g", [128, D], F32)
    a = [P.sb(f"a{i}", [128, D], F32) for i in range(2)]
    b1 = [P.sb(f"b1{i}", [128, D], F32) for i in range(2)]
    b2 = [P.sb(f"b2{i}", [128, D], F32) for i in range(2)]
    ot = [P.sb(f"ot{i}", [128, D], F32) for i in range(2)]
    junk = P.sb("junk", [128, D], BF16)
    ss = [P.sb(f"ss{i}", [128, 1], F32) for i in range(2)]
    P.dma("sp", lambda e: e.dma_start(out=g[:], in_=gf[0:1, :].broadcast_to([128, D])), writes=["g"])
    for ti in range(NT // 128):
        b = ti % 2
        sl = slice(ti * 128, (ti + 1) * 128)
        P.dma("sp", lambda e, b=b, sl=sl: e.dma_start(out=a[b][:], in_=h[sl, :]), writes=[f"a{b}"])
        P.dma("sp", lambda e, b=b, sl=sl: e.dma_start(out=b1[b][:], in_=y1[sl, :]), writes=[f"b1{b}"])
        P.dma("sp", lambda e, b=b, sl=sl: e.dma_start(out=b2[b][:], in_=y2[sl, :]), writes=[f"b2{b}"])
        P.op("pool", lambda e, b=b: e.tensor_tensor(out=b1[b][:], in0=b1[b][:], in1=b2[b][:], op=ALU.add), reads=[f"b1{b}", f"b2{b}"], writes=[f"b1{b}"])
        P.op("dve", lambda e, b=b: e.tensor_tensor(out=a[b][:], in0=a[b][:], in1=b1[b][:], op=ALU.add), reads=[f"a{b}", f"b1{b}"], writes=[f"a{b}"])
        P.op("act", lambda e, b=b: e.activation(out=junk[:], in_=a[b][:], func=AF.Square, accum_out=ss[b][:]), reads=[f"a{b}"], writes=["junk", f"ss{b}"])
        P.op("act", lambda e, b=b: e.activation(out=ss[b][:], in_=ss[b][:], func=AF.Sqrt, bias=1e-6, scale=1.0 / D), reads=[f"ss{b}"], writes=[f"ss{b}"])
        P.op("dve", lambda e, b=b: e.reciprocal(out=ss[b][:], in_=ss[b][:]), reads=[f"ss{b}"], writes=[f"ss{b}"])
        P.op("dve", lambda e, b=b: e.scalar_tensor_tensor(out=ot[b][:], in0=a[b][:], scalar=ss[b][:], in1=g[:], op0=ALU.mult, op1=ALU.mult),
             reads=[f"a{b}", f"ss{b}", "g"], writes=[f"ot{b}"])
        P.dma("pool", lambda e, b=b, sl=sl: e.dma_start(out=out[sl, :], in_=ot[b][:]), reads=[f"ot{b}"], writes=[("out", ti)], is_output=True)
    P.end_phase(final=True)
    return nc


def _run(nc, maps):
    res = run_bass_kernel_spmd(nc, maps, core_ids=list(range(NCORES)))
    return res.results


def kernel(**inp):
    inp = {k: np.asarray(v) for k, v in inp.items()}
    N = NB * S
    r1 = _run(build_l1(), l1_inputs(inp))
    O = np.empty((NB, S, D), dtype=NPBF)
    for c in range(NCORES):
        b, j = c // 4, c % 4
        o = np.asarray(r1[c]["o"])
        if o.dtype != NPBF:
            o = o.view(NPBF)
        O[b, :, 2 * j * 128:(2 * j + 2) * 128] = o[:, 0:256]
        O[b, :, 1024 + j * 256:1024 + (j + 1) * 256] = o[:, 256:512]
    Of = O.reshape(N, D)
    xf = inp["x"].reshape(N, D)
    wr = np.ascontiguousarray(np.concatenate([inp["router_group_w"][0], inp["router_expert_w"][0]], 1))
    br = np.ascontiguousarray(np.concatenate([inp["router_group_b"][0], inp["router_expert_b"][0]])[None])
    io8 = np.arange(8, dtype=np.float32)[None]
    identf = np.eye(128, dtype=np.float32)
    maps = []
    for c in range(NCORES):
        sl = slice(c * NT, (c + 1) * NT)
        maps.append({"oT": np.ascontiguousarray(Of[sl].T).reshape(16, 128, NT), "xs": np.ascontiguousarray(xf[sl]),
                     "wo": np.ascontiguousarray(inp["w_out"][0]), "gf": np.ascontiguousarray(inp["ffn_norm_g"]),
                     "wr": wr, "br": br, "io8": io8, "identf": identf})
    r2 = _run(build_l2a(with_router=True), maps)
    h = np.concatenate([np.asarray(r["h"]) for r in r2], 0)
    hnb = np.concatenate([np.asarray(r["hnb"]) for r in r2], 0)
    if hnb.dtype != NPBF:
        hnb = hnb.view(NPBF)
    ri = np.concatenate([np.asarray(r["ri"]) for r in r2], 0)
    gidx = np.rint(ri[:, 0]).astype(np.int64)
    eloc = np.rint(ri[:, 1:3]).astype(np.int64)
    gates = ri[:, 3:5]
    rows = [[[] for _ in range(8)] for _ in range(8)]
    slot = np.zeros((N, 2), np.int64)
    for k in range(2):
        for g in range(8):
            for e in range(8):
                idx = np.nonzero((gidx == g) & (eloc[:, k] == e))[0]
                base = len(rows[g][e])
                slot[idx, k] = base + np.arange(len(idx))
                rows[g][e].extend([(int(t), k) for t in idx])
    mx = max(len(rows[g][e]) for g in range(8) for e in range(8))
    cap = max(128, ((mx + 127) // 128) * 128)
    ncb = cap // 128
    maps = []
    for g in range(8):
        XT = np.zeros((8, cap, D), dtype=NPBF)
        gc = np.zeros((8, cap), np.float32)
        for e in range(8):
            if rows[g][e]:
                t = np.array([a for a, _ in rows[g][e]]); kk = np.array([b for _, b in rows[g][e]])
                XT[e, :len(t)] = hnb[t]
                gc[e, :len(t)] = gates[t, kk]
        XTt = np.ascontiguousarray(XT.transpose(0, 2, 1)).reshape(8, 16, 128, cap)
        gcol = np.ascontiguousarray(gc.reshape(8, ncb, 128).transpose(2, 0, 1).reshape(128, 8 * ncb))
        maps.append({"XT": XTt, "gcol": gcol,
                     "wg": np.ascontiguousarray(inp["w_gate"][0, g * 8:(g + 1) * 8]), "wu": np.ascontiguousarray(inp["w_up"][0, g * 8:(g + 1) * 8]),
                     "wd": np.ascontiguousarray(inp["w_down"][0, g * 8:(g + 1) * 8])})
    r4 = _run(build_l3(cap), maps)
    Yall = np.stack([np.asarray(r["Y"]) for r in r4], 0)
    y1 = Yall[gidx, eloc[:, 0], slot[:, 0]]
    y2 = Yall[gidx, eloc[:, 1], slot[:, 1]]
    maps = [{"h": np.ascontiguousarray(h[c * NT:(c + 1) * NT]), "y1": np.ascontiguousarray(y1[c * NT:(c + 1) * NT]),
             "y2": np.ascontiguousarray(y2[c * NT:(c + 1) * NT]), "gf": np.ascontiguousarray(inp["final_norm_g"][None])} for c in range(NCORES)]
    r5 = _run(build_l4(), maps)
    out = np.concatenate([np.asarray(r["out"]) for r in r5], 0).reshape(NB, S, D).astype(np.float32)
    return out
```
